# Optimizing a Trainium2 kernel written in Bass

```python
import jax, jax.numpy as jnp
from jax import lax
import numpy as np

D_MODEL = 1024
BATCH = 16
SEQ = 4096
DEPTH = 2

CTX_LEN = 256
GRID_W = 64
HEAD_DIM = 64
N_HEADS = D_MODEL // HEAD_DIM
A_GROUPS = N_HEADS // 4
B_HEADS = (N_HEADS - A_GROUPS) // 2
C_HEADS = N_HEADS - A_GROUPS - B_HEADS
A_WIDTH = A_GROUPS * HEAD_DIM
B_WIDTH = B_HEADS * HEAD_DIM
C_WIDTH = C_HEADS * HEAD_DIM
MIX_WIDTH = A_WIDTH + B_WIDTH + C_WIDTH
A_CHUNK = 128
WIN_ROWS = 8
WIN_COLS = 16
C_CHUNK = 64
C_CONV = 3
D_FF = ((8 * D_MODEL + 3 * 256 - 1) // (3 * 256)) * 256
N_MOD = 6
ROPE_BASE = 10000.0
EPS = 1e-6
SPLITS = (A_WIDTH, A_WIDTH,
          B_WIDTH, B_WIDTH, B_WIDTH,
          C_WIDTH, C_WIDTH, C_WIDTH, C_WIDTH,
          4 * C_HEADS)
IN_COLS = sum(SPLITS)

kernel_name = 'hybrid_gmlp_natten_mlstm_dit_block'


def rms_norm(x, g):
    xf = x.astype(jnp.float32)
    y = xf * lax.rsqrt(jnp.mean(xf * xf, axis=-1, keepdims=True) + EPS)
    return (y * g.astype(jnp.float32)).astype(x.dtype)


def layer_norm(x, g, b):
    xf = x.astype(jnp.float32)
    mu = jnp.mean(xf, axis=-1, keepdims=True)
    var = jnp.mean(jnp.square(xf - mu), axis=-1, keepdims=True)
    y = (xf - mu) * lax.rsqrt(var + EPS)
    return (y * g.astype(jnp.float32) + b.astype(jnp.float32)).astype(x.dtype)


def modulate(x, g, shift, scale):
    return rms_norm(x, g) * (1 + scale) + shift


def split_cols(z):
    return jnp.split(z, np.cumsum(SPLITS)[:-1].tolist(), axis=-1)


def to_heads(z, n_heads):
    b_, n, _ = z.shape
    return z.reshape(b_, n, n_heads, HEAD_DIM).transpose(0, 2, 1, 3)


def from_heads(h):
    b_, h_, n, dh = h.shape
    return h.transpose(0, 2, 1, 3).reshape(b_, n, h_ * dh)


def swiglu(h, w_gate, w_up, w_down):
    return (jax.nn.silu(h @ w_gate) * (h @ w_up)) @ w_down


def chunk_token_mlp(z_u, z_v, ln_g, ln_b, ws, bs):
    b_, n, _ = z_u.shape
    u = jax.nn.gelu(z_u).reshape(b_, n, A_GROUPS, HEAD_DIM)
    v = layer_norm(jax.nn.gelu(z_v).reshape(b_, n, A_GROUPS, HEAD_DIM), ln_g, ln_b)
    v = v.reshape(b_, n // A_CHUNK, A_CHUNK, A_GROUPS, HEAD_DIM)
    s = jnp.einsum('gpq,bcqgd->bcpgd', ws, v) + bs.T[:, :, None]
    return (u * s.reshape(b_, n, A_GROUPS, HEAD_DIM)).reshape(b_, n, A_WIDTH)


def neighbourhood_attention(q, k, v, k_ctx, v_ctx, rpb):
    b_, h_, n, dh = q.shape
    rows = n // GRID_W
    kr = min(WIN_ROWS, rows)
    win = kr * WIN_COLS
    scale = dh ** -0.5
    col = jnp.arange(GRID_W)
    key_col = jnp.clip(col - WIN_COLS // 2, 0, GRID_W - WIN_COLS)[:, None] + jnp.arange(WIN_COLS)
    dc = key_col - col[:, None]
    q_rows = q.reshape(b_, h_, rows, GRID_W, dh).transpose(2, 0, 1, 3, 4)

    def row_block(args):
        r, q_blk = args
        key_row = jnp.clip(r - kr // 2, 0, rows - kr) + jnp.arange(kr)
        idx = (key_row[None, :, None] * GRID_W + key_col[:, None, :]).reshape(GRID_W, win)
        k_win = jnp.take(k, idx, axis=2)
        v_win = jnp.take(v, idx, axis=2)
        bias = rpb[:, (key_row - r)[None, :, None] + WIN_ROWS - 1,
                   dc[:, None, :] + WIN_COLS - 1].reshape(h_, GRID_W, win)
        s_win = jnp.einsum('bhqd,bhqkd->bhqk', q_blk, k_win).astype(jnp.float32) * scale + bias.astype(jnp.float32)
        s_ctx = jnp.einsum('bhqd,bhkd->bhqk', q_blk, k_ctx).astype(jnp.float32) * scale
        p = jax.nn.softmax(jnp.concatenate([s_win, s_ctx], axis=-1), axis=-1).astype(v.dtype)
        return (jnp.einsum('bhqk,bhqkd->bhqd', p[..., :win], v_win)
                + jnp.einsum('bhqk,bhkd->bhqd', p[..., win:], v_ctx))

    out = lax.map(row_block, (jnp.arange(rows), q_rows))
    return out.transpose(1, 2, 0, 3, 4).reshape(b_, h_, n, dh)


def context_attention(q, k, v):
    s = jnp.einsum('bhqd,bhkd->bhqk', q, k).astype(jnp.float32) * (q.shape[-1] ** -0.5)
    p = jax.nn.softmax(s, axis=-1).astype(v.dtype)
    return jnp.einsum('bhqk,bhkd->bhqd', p, v)


def axial_rope(x):
    n = x.shape[1]
    t = jnp.arange(n)
    half = HEAD_DIM // 2
    quarter = half // 2
    inv_freq = ROPE_BASE ** (-jnp.arange(quarter, dtype=jnp.float32) / quarter)

    def rotate(xa, pos):
        ang = pos.astype(jnp.float32)[:, None] * inv_freq
        cos = jnp.cos(ang)[None, :, None, :]
        sin = jnp.sin(ang)[None, :, None, :]
        x1 = xa[..., :quarter].astype(jnp.float32)
        x2 = xa[..., quarter:].astype(jnp.float32)
        return jnp.concatenate([x1 * cos - x2 * sin, x1 * sin + x2 * cos], axis=-1)

    out = jnp.concatenate([rotate(x[..., :half], t // GRID_W), rotate(x[..., half:], t % GRID_W)], axis=-1)
    return out.astype(x.dtype)


def centred_dwconv(x, w, b):
    pad = (C_CONV - 1) // 2
    y = lax.conv_general_dilated(x, w[:, None, :].astype(x.dtype), (1,), [(pad, C_CONV - 1 - pad)],
                                 dimension_numbers=('NWC', 'WIO', 'NWC'), feature_group_count=x.shape[-1])
    return y + b


def mlstm_inputs(zq, zk, zv, zg, conv_w, conv_b, gate_b, with_rope):
    b_, n, _ = zq.shape
    qk = jax.nn.silu(centred_dwconv(jnp.concatenate([zq, zk], axis=-1), conv_w, conv_b))
    q = qk[..., :C_WIDTH].reshape(b_, n, C_HEADS, HEAD_DIM)
    k = qk[..., C_WIDTH:].reshape(b_, n, C_HEADS, HEAD_DIM)
    if with_rope:
        q, k = axial_rope(q), axial_rope(k)
    heads = lambda a: a.transpose(0, 2, 1, 3).astype(jnp.float32)
    g = (zg.reshape(b_, n, 4, C_HEADS).astype(jnp.float32) + gate_b.astype(jnp.float32)).transpose(2, 0, 3, 1)
    fwd = (g[0], jax.nn.log_sigmoid(g[1]))
    bwd = (g[2], jax.nn.log_sigmoid(g[3]))
    v = zv.reshape(b_, n, C_HEADS, HEAD_DIM)
    return heads(q), heads(k) * (HEAD_DIM ** -0.5), heads(v), fwd, bwd


def mlstm_chunk_scan(q, k, v, log_i, log_f, state):
    b_, h_, n, dh = q.shape
    nc = n // C_CHUNK

    def chunks(a):
        return jnp.moveaxis(a.reshape(a.shape[:2] + (nc, C_CHUNK) + a.shape[3:]), 2, 0)

    scan_order = jnp.tril(jnp.ones((C_CHUNK, C_CHUNK), dtype=bool))

    def step(carry, inp):
        c_st, n_st, m_st = carry
        qb, kb, vb, ib, fb = inp
        fcum = jnp.cumsum(fb, axis=-1)
        log_inter = fcum + m_st[..., None]
        log_intra = jnp.where(scan_order, fcum[..., :, None] - fcum[..., None, :] + ib[..., None, :], -jnp.inf)
        m_t = jnp.maximum(log_inter, jnp.max(log_intra, axis=-1))
        w_inter = jnp.exp(log_inter - m_t)
        s = jnp.einsum('bhtd,bhsd->bhts', qb, kb) * jnp.exp(log_intra - m_t[..., None])
        num = w_inter[..., None] * jnp.einsum('bhtk,bhkv->bhtv', qb, c_st) + jnp.einsum('bhts,bhsv->bhtv', s, vb)
        den = w_inter * jnp.einsum('bhtk,bhk->bht', qb, n_st) + jnp.sum(s, axis=-1)
        h = num / jnp.maximum(jnp.abs(den), jnp.exp(-m_t))[..., None]
        log_w = fcum[..., -1:] - fcum + ib
        m_new = jnp.maximum(fcum[..., -1] + m_st, jnp.max(log_w, axis=-1))
        a_prev = jnp.exp(fcum[..., -1] + m_st - m_new)
        a_s = jnp.exp(log_w - m_new[..., None])
        c_new = a_prev[..., None, None] * c_st + jnp.einsum('bhs,bhsk,bhsv->bhkv', a_s, kb, vb)
        n_new = a_prev[..., None] * n_st + jnp.einsum('bhs,bhsk->bhk', a_s, kb)
        return (c_new, n_new, m_new), h

    state, hs = lax.scan(step, state, tuple(chunks(a) for a in (q, k, v, log_i, log_f)))
    return jnp.moveaxis(hs, 0, 2).reshape(b_, h_, n, dh), state


def mlstm_final_state(k, v, log_i, log_f):
    fcum = jnp.cumsum(log_f, axis=-1)
    log_w = fcum[..., -1:] - fcum + log_i
    m = jnp.maximum(fcum[..., -1], jnp.max(log_w, axis=-1))
    a = jnp.exp(log_w - m[..., None])
    return (jnp.einsum('bhs,bhsk,bhsv->bhkv', a, k, v), jnp.einsum('bhs,bhsk->bhk', a, k), m)


def mlstm_bidirectional(lat, ctx, ctx_out):
    q, k, v, (i_f, f_f), (i_b, f_b) = lat
    qc, kc, vc, (ic_f, fc_f), (ic_b, fc_b) = ctx
    rev = lambda a: jnp.flip(a, axis=2)
    if ctx_out:
        b_, h_, _, dh = kc.shape
        zero = (jnp.zeros((b_, h_, dh, dh), jnp.float32), jnp.zeros((b_, h_, dh), jnp.float32),
                jnp.zeros((b_, h_), jnp.float32))
        hc_f, st_f = mlstm_chunk_scan(qc, kc, vc, ic_f, fc_f, zero)
        hc_b, st_b = mlstm_chunk_scan(rev(qc), rev(kc), rev(vc), rev(ic_b), rev(fc_b), zero)
        h_ctx = hc_f + rev(hc_b)
    else:
        st_f = mlstm_final_state(kc, vc, ic_f, fc_f)
        st_b = mlstm_final_state(rev(kc), rev(vc), rev(ic_b), rev(fc_b))
        h_ctx = None
    h_f, _ = mlstm_chunk_scan(q, k, v, i_f, f_f, st_f)
    h_b, _ = mlstm_chunk_scan(rev(q), rev(k), rev(v), rev(i_b), rev(f_b), st_b)
    return h_f + rev(h_b), h_ctx


def hybrid_layer(x, xc, mod, mod_c, norm1_g, w_in, a_ln_g, a_ln_b, a_ws, a_bs, b_rpb,
                 c_conv_w, c_conv_b, c_gate_b, w_out, norm2_g, w_gate, w_up, w_down, ctx_out):
    sh1, sc1, g1, sh2, sc2, g2 = [mod[:, i] for i in range(N_MOD)]
    csh1, csc1, cg1, csh2, csc2, cg2 = [mod_c[i] for i in range(N_MOD)]
    z = modulate(x, norm1_g, sh1, sc1) @ w_in
    zc = modulate(xc, norm1_g, csh1, csc1) @ w_in
    au, av, bq, bk, bv, cq, ck, cv, co, cg = split_cols(z)
    au_c, av_c, bq_c, bk_c, bv_c, cq_c, ck_c, cv_c, co_c, cg_c = split_cols(zc)

    y_a = chunk_token_mlp(au, av, a_ln_g, a_ln_b, a_ws, a_bs)
    k_ctx, v_ctx = to_heads(bk_c, B_HEADS), to_heads(bv_c, B_HEADS)
    y_b = from_heads(neighbourhood_attention(to_heads(bq, B_HEADS), to_heads(bk, B_HEADS),
                                             to_heads(bv, B_HEADS), k_ctx, v_ctx, b_rpb))
    lat_in = mlstm_inputs(cq, ck, cv, cg, c_conv_w, c_conv_b, c_gate_b, True)
    ctx_in = mlstm_inputs(cq_c, ck_c, cv_c, cg_c, c_conv_w, c_conv_b, c_gate_b, False)
    h_lat, h_ctx = mlstm_bidirectional(lat_in, ctx_in, ctx_out)
    y_c = from_heads(h_lat).astype(x.dtype) * jax.nn.sigmoid(co)

    x = x + g1 * (jnp.concatenate([y_a, y_b, y_c], axis=-1) @ w_out)
    x = x + g2 * swiglu(modulate(x, norm2_g, sh2, sc2), w_gate, w_up, w_down)

    if ctx_out:
        y_ac = chunk_token_mlp(au_c, av_c, a_ln_g, a_ln_b, a_ws, a_bs)
        y_bc = from_heads(context_attention(to_heads(bq_c, B_HEADS), k_ctx, v_ctx))
        y_cc = from_heads(h_ctx).astype(xc.dtype) * jax.nn.sigmoid(co_c)
        xc = xc + cg1 * (jnp.concatenate([y_ac, y_bc, y_cc], axis=-1) @ w_out)
        xc = xc + cg2 * swiglu(modulate(xc, norm2_g, csh2, csc2), w_gate, w_up, w_down)
    return x, xc


def setup_inputs(seed: int = 0) -> dict:
    key = jax.random.key(seed)
    ks = jax.random.split(key, 24)
    f32 = jnp.float32
    nrm = lambda k, shape, s: jax.random.normal(k, shape, f32) * s
    gate_i = nrm(ks[14], (DEPTH, 2, C_HEADS), 0.1)
    gate_f = jnp.linspace(3.0, 6.0, C_HEADS, dtype=f32) + nrm(ks[15], (DEPTH, 2, C_HEADS), 0.1)
    c_gate_b = jnp.stack([gate_i[:, 0], gate_f[:, 0], gate_i[:, 1], gate_f[:, 1]], axis=1)
    return {
        'x': nrm(ks[0], (BATCH, SEQ, D_MODEL), 1.0),
        'c': nrm(ks[1], (BATCH, D_MODEL), 1.0),
        'ctx': nrm(ks[2], (BATCH, CTX_LEN, D_MODEL), 1.0),
        'c_ctx': nrm(ks[3], (D_MODEL,), 1.0),
        'w_mod': nrm(ks[4], (DEPTH, D_MODEL, N_MOD * D_MODEL), 0.5 * D_MODEL ** -0.5),
        'b_mod': nrm(ks[5], (DEPTH, N_MOD * D_MODEL), 0.02),
        'norm1_g': 1.0 + nrm(ks[6], (DEPTH, D_MODEL), 0.02),
        'w_in': nrm(ks[7], (DEPTH, D_MODEL, IN_COLS), D_MODEL ** -0.5),
        'a_ln_g': 1.0 + nrm(ks[8], (DEPTH, A_GROUPS, HEAD_DIM), 0.02),
        'a_ln_b': nrm(ks[9], (DEPTH, A_GROUPS, HEAD_DIM), 0.02),
        'a_ws': nrm(ks[10], (DEPTH, A_GROUPS, A_CHUNK, A_CHUNK), A_CHUNK ** -0.5),
        'a_bs': 1.0 + nrm(ks[11], (DEPTH, A_GROUPS, A_CHUNK), 0.02),
        'b_rpb': nrm(ks[12], (DEPTH, B_HEADS, 2 * WIN_ROWS - 1, 2 * WIN_COLS - 1), 0.02),
        'c_conv_w': nrm(ks[13], (DEPTH, C_CONV, 2 * C_WIDTH), C_CONV ** -0.5),
        'c_conv_b': nrm(ks[16], (DEPTH, 2 * C_WIDTH), 0.02),
        'c_gate_b': c_gate_b,
        'w_out': nrm(ks[17], (DEPTH, MIX_WIDTH, D_MODEL), MIX_WIDTH ** -0.5),
        'norm2_g': 1.0 + nrm(ks[18], (DEPTH, D_MODEL), 0.02),
        'w_gate': nrm(ks[19], (DEPTH, D_MODEL, D_FF), D_MODEL ** -0.5),
        'w_up': nrm(ks[20], (DEPTH, D_MODEL, D_FF), D_MODEL ** -0.5),
        'w_down': nrm(ks[21], (DEPTH, D_FF, D_MODEL), D_FF ** -0.5),
        'final_g': 1.0 + nrm(ks[22], (D_MODEL,), 0.02),
    }


def reference(x, c, ctx, c_ctx, w_mod, b_mod, norm1_g, w_in, a_ln_g, a_ln_b, a_ws, a_bs, b_rpb,
              c_conv_w, c_conv_b, c_gate_b, w_out, norm2_g, w_gate, w_up, w_down, final_g):
    b_, _, d = x.shape
    xc = ctx
    for l in range(DEPTH):
        mod = (jax.nn.silu(c) @ w_mod[l] + b_mod[l]).reshape(b_, N_MOD, 1, d)
        mod_c = (jax.nn.silu(c_ctx) @ w_mod[l] + b_mod[l]).reshape(N_MOD, d)
        x, xc = hybrid_layer(x, xc, mod, mod_c, norm1_g[l], w_in[l], a_ln_g[l], a_ln_b[l], a_ws[l], a_bs[l],
                             b_rpb[l], c_conv_w[l], c_conv_b[l], c_gate_b[l], w_out[l], norm2_g[l],
                             w_gate[l], w_up[l], w_down[l], l < DEPTH - 1)
    return rms_norm(x, final_g)
```

```python
import bisect
import contextlib
import numpy as np
import concourse.bass as bass
import concourse.mybir as mybir
from concourse.bass_utils import run_bass_kernel_spmd

F32 = mybir.dt.float32
BF16 = mybir.dt.bfloat16
AF = mybir.ActivationFunctionType
ALU = mybir.AluOpType
AX = mybir.AxisListType

ENGINES = ("pe", "act", "dve", "pool", "sp")

D = 1024
NCH = 8
SEQ = 4096
CTX = 256
T = SEQ + CTX
NT = T // 128
DEPTH = 2
NBL = 2
DFF = 2816
NFF = DFF // 128
INC = 3224
EPS = 1e-6
GW = 64
NEG = -30000.0
O_AU, O_AV, O_BQ, O_BK, O_BV, O_CQ, O_CK, O_CV, O_CO, O_CG = 0, 256, 512, 896, 1280, 1664, 2048, 2432, 2816, 3200
BLOCKS = [(0, 256)] + [(256 + 512 * i, 512) for i in range(8)]


class Buf:
    __slots__ = ("name", "lastw", "readers", "sem", "semname", "psum")

    def __init__(self, name, semname=None):
        self.psum = False
        self.name = name
        self.semname = semname or name
        self.lastw = {}
        self.readers = {}
        self.sem = None


class Op:
    __slots__ = ("idx", "eng", "fn", "deps", "is_dma", "sem", "signal")

    def __init__(self, idx, eng, fn, is_dma):
        self.idx = idx; self.eng = eng; self.fn = fn; self.is_dma = is_dma
        self.deps = {}
        self.sem = None; self.signal = False


class VW:
    __slots__ = ("ap", "b", "key")

    def __init__(self, ap, b, key=None):
        self.ap = ap; self.b = b; self.key = key

    def k(self, key):
        return VW(self.ap, self.b, key)


class TL:
    def __init__(self, t, b):
        self.t = t; self.b = b

    def __getitem__(self, idx):
        return VW(self.t[idx], self.b)

    def v(self, ap, key=None):
        return VW(ap, self.b, key)


class Prog:
    def __init__(self, nc):
        self.nc = nc
        self.ops = []
        self.dma_sem_names = []
        self.nbuf = 0
        self.last_on = {e: None for e in ENGINES}
        self.dma_ops = []
        self.strict_war = False

    def buf(self, name=None, semname=None):
        self.nbuf += 1
        return Buf(name or f"b{self.nbuf}", semname)

    def _dep(self, op, idx, strong):
        if idx == op.idx:
            return
        op.deps[idx] = op.deps.get(idx, False) or strong

    def _track(self, op, reads, writes):
        for v in reads:
            b, k = v.b, v.key
            if b.psum:
                for rs in b.readers.values():
                    for r in rs:
                        if self.ops[r].eng != op.eng:
                            self._dep(op, r, True)
            if k is None:
                for w in b.lastw.values(): self._dep(op, w, True)
            else:
                if k in b.lastw: self._dep(op, b.lastw[k], True)
                if None in b.lastw: self._dep(op, b.lastw[None], True)
        for v in writes:
            b, k = v.b, v.key
            if k is None:
                for w in b.lastw.values(): self._dep(op, w, True)
                for rs in b.readers.values():
                    for r in rs: self._dep(op, r, False)
            else:
                for kk in (k, None):
                    if kk in b.lastw: self._dep(op, b.lastw[kk], True)
                    if kk in b.readers:
                        for r in b.readers[kk]: self._dep(op, r, False)
        for v in reads:
            v.b.readers.setdefault(v.key, []).append(op.idx)
        for v in writes:
            b, k = v.b, v.key
            if k is None:
                b.lastw = {None: op.idx}; b.readers = {}
            else:
                b.lastw[k] = op.idx; b.readers[k] = []

    def op(self, eng, fn, reads=(), writes=()):
        o = Op(len(self.ops), eng, fn, False)
        self.ops.append(o)
        self._track(o, reads, writes)
        self.last_on[eng] = o.idx
        return o

    def dma(self, queue, out, in_, **kw):
        o = Op(len(self.ops), queue, None, True)
        oap, iap = out.ap, in_.ap

        def fn(eng, oap=oap, iap=iap, kw=kw):
            return eng.dma_start(out=oap, in_=iap, **kw)
        o.fn = fn
        self.ops.append(o)
        wb = out.b
        if wb.sem is None:
            nm = "d_" + wb.semname
            if nm not in self.dma_sem_names:
                self.dma_sem_names.append(nm)
            wb.sem = self.dma_sem_names.index(nm)
        o.sem = wb.sem
        self._track(o, [in_], [out])
        self.dma_ops.append(o.idx)
        return o

    def fence(self, eng, reads):
        o = Op(len(self.ops), eng, None, False)
        self.ops.append(o)
        self._track(o, reads, ())
        return o

    def barrier(self):
        lasts = [i for i in self.last_on.values() if i is not None]
        dm = list(self.dma_ops)
        for e in ENGINES:
            o = Op(len(self.ops), e, None, False)
            self.ops.append(o)
            for i in lasts: self._dep(o, i, True)
            for i in dm: self._dep(o, i, True)
        self.dma_ops = []

    def emit(self):
        nc = self.nc
        ops = self.ops

        def needed(o, p, strong):
            if p.is_dma:
                return True
            if p.fn is None:
                return False
            if p.eng == o.eng and not o.is_dma:
                if o.eng == "pe":
                    return False
                return strong or self.strict_war
            return True

        red = {}
        for o in ops:
            best = {}
            dmas = {}
            for d, strong in o.deps.items():
                p = ops[d]
                if not needed(o, p, strong):
                    continue
                if p.is_dma:
                    dmas[p.sem] = p
                elif p.eng not in best or best[p.eng].idx < p.idx:
                    best[p.eng] = p
            red[o.idx] = list(best.values()) + list(dmas.values())
            for p in best.values():
                p.signal = True
        cnt = {e: 0 for e in ENGINES}
        sigval = {}
        EPOCH = 30000
        for o in ops:
            if o.is_dma or o.fn is None:
                continue
            if o.signal:
                cnt[o.eng] += 1
            c = cnt[o.eng]
            sigval[o.idx] = (0, 0) if c == 0 else ((c - 1) // EPOCH, (c - 1) % EPOCH + 1)
        nepoch = {e: max(1, (cnt[e] + EPOCH - 1) // EPOCH) for e in ENGINES}
        dma_lists = [[] for _ in self.dma_sem_names]
        for o in ops:
            if o.is_dma:
                dma_lists[o.sem].append(o.idx)
        self.stats = dict(n_ops=len(ops), sig=dict(cnt), n_dma_sems=len(self.dma_sem_names))
        with contextlib.ExitStack() as es:
            esems = {(e, k): es.enter_context(nc.semaphore(f"s_{e}{k}"))
                     for e in ("pe", "act", "dve", "pool") for k in range(nepoch[e])}
            dsems = [es.enter_context(nc.semaphore(n)) for n in self.dma_sem_names]
            block = es.enter_context(nc.Block())
            per_eng = {e: [o for o in ops if o.eng == e] for e in ENGINES}

            def run(engname, eng):
                seen = {}
                nwait = 0
                for o in per_eng[engname]:
                    waits = {}
                    for p in red[o.idx]:
                        if p.is_dma:
                            n = bisect.bisect_left(dma_lists[p.sem], o.idx)
                            key = ("d", p.sem); val = 16 * n
                        else:
                            ep, val = sigval[p.idx]
                            key = ("e", p.eng, ep)
                        if val > waits.get(key, 0):
                            waits[key] = val
                    for key, val in waits.items():
                        if seen.get(key, 0) >= val:
                            continue
                        seen[key] = val
                        sem = dsems[key[1]] if key[0] == "d" else esems[(key[1], key[2])]
                        eng.wait_ge(sem, val)
                        nwait += 1
                    if o.fn is None:
                        continue
                    ins = o.fn(eng)
                    if o.is_dma:
                        ins.then_inc(dsems[o.sem], 16)
                    elif o.signal:
                        ins.then_inc(esems[(engname, sigval[o.idx][0])], 1)
                self.stats["waits_" + engname] = nwait

            @block.tensor
            def _(eng): run("pe", eng)

            @block.scalar
            def _(eng): run("act", eng)

            @block.vector
            def _(eng): run("dve", eng)

            @block.gpsimd
            def _(eng): run("pool", eng)

            @block.sync
            def _(eng): run("sp", eng)


_DT_SIZE = {F32: 4, BF16: 2}


class Arena:
    def __init__(self, nc, P, base=17408, limit=212000):
        self.nc = nc; self.P = P; self.off = base; self.limit = limit; self.n = 0; self.peak = 0

    def alloc(self, name, shape, dtype):
        nbytes = int(np.prod(shape[1:])) * _DT_SIZE[dtype]
        self.off = (self.off + 63) // 64 * 64
        self.n += 1
        t = self.nc.alloc_sbuf_tensor_at(f"{name}_{self.n}", list(shape), dtype, offset=self.off)
        self.off += nbytes
        self.peak = max(self.peak, self.off)
        assert self.off <= self.limit, f"SBUF arena overflow at {name}: {self.off}"
        return TL(t, self.P.buf(f"{name}_{self.n}", semname=name))

    def mark(self):
        return self.off

    def reset(self, m):
        self.off = m


class K:
    def __init__(self, debug=None, upto=None):
        self.debug = debug or []
        self.upto = upto
        self.nb_run = 1 if "oneb" in self.debug else NBL
        nc = self.nc = bass.Bass("TRN2", target_bir_lowering=False)
        P = self.P = Prog(nc)
        self.ar = Arena(nc, P)
        self.din = {}
        self.dbg_out = {}

    def mm(self, out, lhsT, rhs, start=True, stop=True):
        oa, la, ra = out.ap, lhsT.ap, rhs.ap
        self.P.op("pe", lambda e: e.matmul(oa, lhsT=la, rhs=ra, start=start, stop=stop),
                  reads=[lhsT, rhs], writes=[out])

    def act(self, out, in_, func, bias=None, scale=None, eng="act", accum_out=None):
        oa, ia = out.ap, in_.ap
        kw = {}
        reads = [in_]
        if bias is not None:
            if isinstance(bias, VW):
                kw["bias"] = bias.ap; reads.append(bias)
            else:
                kw["bias"] = float(bias)
        if scale is not None:
            if isinstance(scale, VW):
                kw["scale"] = scale.ap; reads.append(scale)
            else:
                kw["scale"] = float(scale)
        writes = [out]
        if accum_out is not None:
            kw["accum_out"] = accum_out.ap; writes.append(accum_out)
        self.P.op("act", lambda e: e.activation(out=oa, in_=ia, func=func, **kw), reads=reads, writes=writes)

    def tt(self, eng, out, in0, in1, op):
        oa, a, b = out.ap, in0.ap, in1.ap
        self.P.op(eng, lambda e: e.tensor_tensor(out=oa, in0=a, in1=b, op=op), reads=[in0, in1], writes=[out])

    def ts(self, eng, out, in0, s1, op0, s2=None, op1=None):
        oa, a = out.ap, in0.ap
        reads = [in0]
        if isinstance(s1, VW):
            reads.append(s1); s1v = s1.ap
        else:
            s1v = float(s1)
        if isinstance(s2, VW):
            reads.append(s2); s2v = s2.ap
        else:
            s2v = None if s2 is None else float(s2)
        if op1 is None and eng == "pool" and op0 == ALU.mult:
            self.P.op(eng, lambda e: e.tensor_scalar(out=oa, in0=a, scalar1=s1v, scalar2=0.0, op0=ALU.mult, op1=ALU.add),
                      reads=reads, writes=[out])
        elif op1 is None:
            self.P.op(eng, lambda e: e.tensor_scalar(out=oa, in0=a, scalar1=s1v, scalar2=None, op0=op0),
                      reads=reads, writes=[out])
        else:
            self.P.op(eng, lambda e: e.tensor_scalar(out=oa, in0=a, scalar1=s1v, scalar2=s2v, op0=op0, op1=op1),
                      reads=reads, writes=[out])

    def stt(self, out, in0, scalar, in1, op0, op1):
        oa, a, b = out.ap, in0.ap, in1.ap
        reads = [in0, in1]
        if isinstance(scalar, VW):
            reads.append(scalar); sv = scalar.ap
        else:
            sv = float(scalar)
        self.P.op("dve", lambda e: e.scalar_tensor_tensor(out=oa, in0=a, scalar=sv, in1=b, op0=op0, op1=op1),
                  reads=reads, writes=[out])

    def cp(self, eng, out, in_):
        oa, ia = out.ap, in_.ap
        if eng == "act":
            self.P.op("act", lambda e: e.activation(out=oa, in_=ia, func=AF.Copy), reads=[in_], writes=[out])
        else:
            self.P.op(eng, lambda e: e.tensor_copy(out=oa, in_=ia), reads=[in_], writes=[out])

    def memset(self, eng, out, val):
        oa = out.ap
        self.P.op(eng, lambda e: e.memset(oa, float(val)), reads=[], writes=[out])

    def recip(self, out, in_):
        oa, ia = out.ap, in_.ap
        self.P.op("dve", lambda e: e.reciprocal(out=oa, in_=ia), reads=[in_], writes=[out])

    def rsum(self, out, in_, axis=AX.X):
        oa, ia = out.ap, in_.ap
        self.P.op("dve", lambda e: e.reduce_sum(out=oa, in_=ia, axis=axis), reads=[in_], writes=[out])

    def dma(self, q, out, in_, **kw):
        self.P.dma(q, out, in_, **kw)

    def dram_in(self, name, shape, dtype=F32):
        t = self.nc.dram_tensor(name, list(shape), dtype, kind="ExternalInput")
        tl = TL(t.ap(), self.P.buf(name))
        self.din[name] = tl
        return tl

    def dram_scr(self, name, shape, dtype):
        kind = "ExternalOutput" if name in self.debug else "Internal"
        t = self.nc.dram_tensor(name, list(shape), dtype, kind=kind)
        tl = TL(t.ap(), self.P.buf(name))
        if name in self.debug:
            self.dbg_out[name] = tl
        return tl

    def build(self):
        nc, P, ar = self.nc, self.P, self.ar
        x_in = self.dram_in("x", [NBL, SEQ, D])
        ctx_in = self.dram_in("ctx", [NBL, CTX, D])
        cT_in = self.dram_in("cT", [128, NCH, 3])
        wmod_in = self.dram_in("w_mod", [DEPTH, D, 6 * D])
        bmod_in = self.dram_in("bmodT", [128, DEPTH, 6, NCH])
        n1g_in = self.dram_in("n1gT", [128, DEPTH, NCH])
        n2g_in = self.dram_in("n2gT", [128, DEPTH, NCH])
        fg_in = self.dram_in("fgT", [128, NCH])
        win_in = self.dram_in("w_in", [DEPTH, D, INC])
        lng_in = self.dram_in("lngB", [DEPTH, 128, 256])
        lnb_in = self.dram_in("lnbB", [DEPTH, 128, 256])
        wsT_in = self.dram_in("wsT", [DEPTH, 128, 4, 128])
        bsB_in = self.dram_in("bsB", [DEPTH, 128, 2, 128])
        wout_in = self.dram_in("w_out", [DEPTH, D, D])
        wg_in = self.dram_in("w_gate", [DEPTH, D, DFF])
        wu_in = self.dram_in("w_up", [DEPTH, D, DFF])
        wd_in = self.dram_in("w_down", [DEPTH, DFF, D])
        idf_in = self.dram_in("identF", [128, 128])
        bias_in = self.dram_in("biasT", [DEPTH, 6, 20, 128, 512])
        cst = dict(antiF=self.dram_in("antiF", [128, 128]), maskf=self.dram_in("maskf", [128, 128]),
                   maskb=self.dram_in("maskb", [128, 128]), sel=self.dram_in("sel", [128, 12, 128]),
                   mk=self.dram_in("mk", [12, 2]), gb=self.dram_in("gb", [12, DEPTH, 2]),
                   rotT=self.dram_in("rotT", [128, 128]), convW=self.dram_in("convW", [128, DEPTH, 2, 3, 3]),
                   convB=self.dram_in("convB", [128, DEPTH, 2, 3]), cosT=self.dram_in("cosT", [128, SEQ]),
                   sinT=self.dram_in("sinT", [128, SEQ]))
        out_t = nc.dram_tensor("out", [NBL, SEQ, D], F32, kind="ExternalOutput")
        out_d = TL(out_t.ap(), P.buf("out"))
        self.out_d = out_d

        xT = [self.dram_scr(f"xT{b}", [NCH, 128, T], F32) for b in range(NBL)]
        yT = [self.dram_scr(f"yT{b}", [NCH, 128, T], BF16) for b in range(NBL)]
        qB = [self.dram_scr(f"qB{b}", [3, 128, T], BF16) for b in range(NBL)]
        kB = [self.dram_scr(f"kB{b}", [3, 128, T], BF16) for b in range(NBL)]
        vB = [self.dram_scr(f"vB{b}", [T, 6, 65], BF16) for b in range(NBL)]
        zqC = [self.dram_scr(f"zqC{b}", [3, 128, T], F32) for b in range(NBL)]
        zkC = [self.dram_scr(f"zkC{b}", [3, 128, T], F32) for b in range(NBL)]
        vC = [self.dram_scr(f"vC{b}", [T, 6, 65], BF16) for b in range(NBL)]
        oC = [self.dram_scr(f"oC{b}", [3, 128, T], BF16) for b in range(NBL)]
        gC = [self.dram_scr(f"gC{b}", [T, 24], F32) for b in range(NBL)]
        self.scr = dict(xT=xT, yT=yT, qB=qB, kB=kB, vB=vB, zqC=zqC, zkC=zkC, vC=vC, oC=oC, gC=gC)

        ps = [TL(nc.alloc_psum_tensor(f"ps{i}", [128, 512], F32), P.buf(f"ps{i}")) for i in range(8)]
        for p_ in ps:
            p_.b.psum = True
        self.ps = ps

        identF = ar.alloc("identF", [128, 128], F32)
        identB = ar.alloc("identB", [128, 128], BF16)
        onesB = ar.alloc("onesB", [128, 128], BF16)
        self.dma("sp", identF[:], idf_in[:])
        self.cp("dve", identB[:], identF[:])
        self.memset("dve", onesB[:], 1.0)
        self.identF, self.identB, self.onesB = identF, identB, onesB
        modS = ar.alloc("modS", [128, DEPTH, 6, NCH, 3], F32)
        modA = ar.alloc("modA", [128, DEPTH, 2, NCH, 3], F32)
        n1g = ar.alloc("n1g", [128, DEPTH, NCH], F32)
        n2g = ar.alloc("n2g", [128, DEPTH, NCH], F32)
        fgT = ar.alloc("fgT", [128, NCH], F32)
        bmodT = ar.alloc("bmodT", [128, DEPTH, 6, NCH], F32)
        self.dma("sp", n1g[:], n1g_in[:]); self.dma("sp", n2g[:], n2g_in[:])
        self.dma("sp", fgT[:], fg_in[:]); self.dma("sp", bmodT[:], bmod_in[:])
        self.modS, self.modA, self.fgT = modS, modA, fgT
        base_mark = ar.mark()

        if "skip01" in self.debug:
            for l in range(DEPTH):
                P.barrier(); ar.reset(base_mark)
                if "skipB" not in self.debug:
                    self.phase_B(l, bias_in)
                    if self.upto == f"B_{l}":
                        return self.finish()
                P.barrier(); ar.reset(base_mark)
                self.phase_C(l, cst)
                if self.upto in (f"C_{l}", f"C2_{l}", f"C1_{l}", f"C2a_{l}", f"C2b_{l}", f"C2c_{l}", f"C2d_{l}"):
                    return self.finish()
        scT = ar.alloc("scT", [128, NCH, 3], F32)
        self.dma("sp", scT[:], cT_in[:])
        self.act(scT[:], scT[:], AF.Silu)
        wm = [ar.alloc(f"wm{i}", [128, NCH, D], F32) for i in range(2)]
        it = 0
        for l in range(DEPTH):
            for i in range(6):
                w = wm[it % 2]
                src = wmod_in.v(wmod_in.t[l, :, i * D:(i + 1) * D].rearrange("(k p) n -> p k n", p=128))
                self.dma("sp", w[:], src)
                pst = ps[it % 2]
                for j in range(NCH):
                    for k in range(NCH):
                        self.mm(pst.v(pst.t[:, j * 3:(j + 1) * 3]), w.v(w.t[:, k, j * 128:(j + 1) * 128]),
                                scT.v(scT.t[:, k, :]), start=(k == 0), stop=(k == NCH - 1))
                for j in range(NCH):
                    self.ts("dve", modS.v(modS.t[:, l, i, j, :]), pst.v(pst.t[:, j * 3:(j + 1) * 3]),
                            bmodT.v(bmodT.t[:, l, i, j:j + 1]), ALU.add)
                it += 1
            for nrm, (sci, gt) in enumerate([(1, n1g), (4, n2g)]):
                for j in range(NCH):
                    self.ts("dve", modA.v(modA.t[:, l, nrm, j, :]), modS.v(modS.t[:, l, sci, j, :]),
                            1.0, ALU.add, gt.v(gt.t[:, l, j:j + 1]), ALU.mult)
        if "modS" in self.debug:
            d = self.dram_scr("modS", [128, DEPTH * 6 * NCH * 3], F32)
            self.dma("sp", d[:], modS.v(modS.t[:].rearrange("p a b c d -> p (a b c d)")))
        P.barrier()
        ar.reset(base_mark)

        self.phase_T(x_in, ctx_in, xT)
        if self.upto == "T":
            return self.finish()
        for l in range(DEPTH):
            P.barrier(); ar.reset(base_mark)
            self.phase_1(l, win_in, lng_in, lnb_in, wsT_in, bsB_in)
            if self.upto == f"1_{l}":
                return self.finish()
            P.barrier(); ar.reset(base_mark)
            if "skipB" not in self.debug:
                self.phase_B(l, bias_in)
            if self.upto == f"B_{l}":
                return self.finish()
            P.barrier(); ar.reset(base_mark)
            self.phase_C(l, cst)
            if self.upto in (f"C_{l}", f"C2_{l}", f"C1_{l}", f"C2a_{l}", f"C2b_{l}", f"C2c_{l}", f"C2d_{l}"):
                return self.finish()
            P.barrier(); ar.reset(base_mark)
            if f"inject_y{l}" in self.debug:
                yin = self.dram_in("yinj", [NBL, NCH, 128, T], BF16)
                for b in range(self.nb_run):
                    self.dma("sp", yT[b][:], yin.v(yin.t[b]))
                P.barrier()
            self.phase_4a(l, wout_in)
            P.barrier(); ar.reset(base_mark)
            self.phase_4b(l, wg_in, wu_in, wd_in)
            if self.upto == f"4_{l}":
                return self.finish()
        return self.finish()

    def phase_4a(self, l, wout_in):
        ar, ps, S = self.ar, self.ps, self.scr
        wo = ar.alloc("wo", [128, NCH, D], BF16)
        for k in range(NCH):
            self.dma("pool", wo.v(wo.t[:, k, :], key=k), wout_in.v(wout_in.t[l, k * 128:(k + 1) * 128, :]),
                     max_dma_last_dim=4096)
        xb = [ar.alloc(f"xb{i}", [128, NCH, 512], F32) for i in range(2)]
        yb = [ar.alloc(f"yb{i}", [128, NCH, 512], BF16) for i in range(2)]
        it = 0
        items = [(b, s0, n) for b in range(self.nb_run) for (s0, n) in BLOCKS if not (l == DEPTH - 1 and s0 < CTX)]

        def load(i):
            b_, s0_, n_ = items[i]
            xt_, yt_ = xb[i % 2], yb[i % 2]
            xTd_, yTd_ = S["xT"][b_], S["yT"][b_]
            self.dma("sp", yt_.v(yt_.t[:, :, :n_]), yTd_.v(yTd_.t[:, :, s0_:s0_ + n_].rearrange("j p t -> p j t")))
            self.dma("sp", xt_.v(xt_.t[:, :, :n_]), xTd_.v(xTd_.t[:, :, s0_:s0_ + n_].rearrange("j p t -> p j t")))
        load(0)
        for (b, s0, n) in items:
            if True:
                src = 2 if s0 < CTX else b
                xbt, ybt = xb[it % 2], yb[it % 2]
                xTd, yTd = S["xT"][b], S["yT"][b]
                if it + 1 < len(items):
                    load(it + 1)
                for j in range(NCH):
                    pst = ps[j % 4]
                    for k in range(NCH):
                        self.mm(pst.v(pst.t[:, :n]), wo.v(wo.t[:, k, j * 128:(j + 1) * 128]), ybt.v(ybt.t[:, k, :n]),
                                start=(k == 0), stop=(k == NCH - 1))
                    self.stt(xbt.v(xbt.t[:, j, :n]), pst.v(pst.t[:, :n]), self.modS.v(self.modS.t[:, l, 2, j, src:src + 1]),
                             xbt.v(xbt.t[:, j, :n]), ALU.mult, ALU.add)
                self.dma("sp", xTd.v(xTd.t[:, :, s0:s0 + n].rearrange("j p t -> p j t"), key=s0), xbt.v(xbt.t[:, :, :n]))
                it += 1

    def phase_4b(self, l, wg_in, wu_in, wd_in):
        ar, ps, S = self.ar, self.ps, self.scr
        last = (l == DEPTH - 1)
        NB = 256
        wg = ar.alloc("wg", [128, NCH, DFF], BF16)
        wu = ar.alloc("wu", [128, NCH, DFF], BF16)
        wd = ar.alloc("wd", [128, NFF, D], BF16)
        for k in range(NCH):
            self.dma("pool", wg.v(wg.t[:, k, :], key=k), wg_in.v(wg_in.t[l, k * 128:(k + 1) * 128, :]), max_dma_last_dim=4096)
            self.dma("pool", wu.v(wu.t[:, k, :], key=k), wu_in.v(wu_in.t[l, k * 128:(k + 1) * 128, :]), max_dma_last_dim=4096)
        for f in range(NFF):
            self.dma("pool", wd.v(wd.t[:, f, :], key=f), wd_in.v(wd_in.t[l, f * 128:(f + 1) * 128, :]), max_dma_last_dim=4096)
        xb = [ar.alloc(f"xb{i}", [128, NCH, NB], F32) for i in range(2)]
        xm = [ar.alloc(f"xm{i}", [128, NCH, NB], BF16) for i in range(2)]
        sqb = [ar.alloc(f"sqb{i}", [128, NB], BF16) for i in range(2)]
        tmp = [ar.alloc(f"tmp{i}", [128, NB], F32) for i in range(2)]
        rstd = [ar.alloc(f"rstd{i}", [128, NB], F32) for i in range(2)]
        rstf = ar.alloc("rstf", [128, NB], F32)
        sg = [ar.alloc(f"sg{i}", [128, NB], F32) for i in range(2)]
        h = ar.alloc("h", [128, NFF, NB], BF16)
        ost = [ar.alloc(f"ost{i}", [128, D], F32) for i in range(2)] if last else None
        n = NB
        items = [(b, s0) for b in range(self.nb_run) for s0 in range(0, T, NB) if not (last and s0 < CTX)]

        def load(i):
            b_, s0_ = items[i]
            xTd_ = S["xT"][b_]
            self.dma("sp", xb[i % 2][:], xTd_.v(xTd_.t[:, :, s0_:s0_ + n].rearrange("j p t -> p j t")))

        def srcof(i):
            b_, s0_ = items[i]
            return 2 if s0_ < CTX else b_
        oc = 0
        load(0)
        self.norm_mod(xb[0], n, l, 1, srcof(0), xm[0], sqb, rstd[0], tmp)
        for it, (b, s0) in enumerate(items):
            src = srcof(it)
            xbt, xmt = xb[it % 2], xm[it % 2]
            xTd = S["xT"][b]
            nxt = it + 1 < len(items)
            if nxt:
                load(it + 1)
            for f in range(NFF):
                pg, pu = ps[(2 * f) % 6], ps[(2 * f + 1) % 6]
                for k in range(NCH):
                    self.mm(pg.v(pg.t[:, :n]), wg.v(wg.t[:, k, f * 128:(f + 1) * 128]), xmt.v(xmt.t[:, k, :]),
                            start=(k == 0), stop=(k == NCH - 1))
                for k in range(NCH):
                    self.mm(pu.v(pu.t[:, :n]), wu.v(wu.t[:, k, f * 128:(f + 1) * 128]), xmt.v(xmt.t[:, k, :]),
                            start=(k == 0), stop=(k == NCH - 1))
                sgt = sg[f % 2]
                self.act(sgt[:], pg.v(pg.t[:, :n]), AF.Silu)
                self.tt("dve", h.v(h.t[:, f, :]), pu.v(pu.t[:, :n]), sgt[:], ALU.mult)
            if nxt:
                self.norm_stats(xb[(it + 1) % 2], n, sqb, rstd[(it + 1) % 2])
            for j in range(NCH):
                pst = ps[j % 6]
                for f in range(NFF):
                    self.mm(pst.v(pst.t[:, :n]), wd.v(wd.t[:, f, j * 128:(j + 1) * 128]), h.v(h.t[:, f, :]),
                            start=(f == 0), stop=(f == NFF - 1))
                if nxt:
                    self.norm_apply(xb[(it + 1) % 2], n, l, 1, srcof(it + 1), xm[(it + 1) % 2], rstd[(it + 1) % 2], tmp, j)
                self.stt(xbt.v(xbt.t[:, j, :]), pst.v(pst.t[:, :n]), self.modS.v(self.modS.t[:, l, 5, j, src:src + 1]),
                         xbt.v(xbt.t[:, j, :]), ALU.mult, ALU.add)
            if not last:
                self.dma("sp", xTd.v(xTd.t[:, :, s0:s0 + n].rearrange("j p t -> p j t"), key=s0), xbt[:])
            else:
                self.norm_stats(xbt, n, sqb, rstf)
                for j in range(NCH):
                    self.stt(xbt.v(xbt.t[:, j, :]), xbt.v(xbt.t[:, j, :]), self.fgT.v(self.fgT.t[:, j:j + 1]),
                             rstf[:], ALU.mult, ALU.mult)
                for sub in range(n // 128):
                    o = ost[oc % 2]; oc += 1
                    for half in range(2):
                        pst = ps[6 if half == 0 else (oc % 6)]
                        for jj in range(4):
                            j = half * 4 + jj
                            self.mm(pst.v(pst.t[:, jj * 128:(jj + 1) * 128]),
                                    xbt.v(xbt.t[:, j, sub * 128:(sub + 1) * 128]), self.identF[:])
                        self.cp("act" if half == 0 else "dve", o.v(o.t[:, half * 512:(half + 1) * 512]), pst[:])
                    t0 = s0 - CTX + sub * 128
                    self.dma("sp", self.out_d.v(self.out_d.t[b, t0:t0 + 128, :], key=(b, t0)), o[:])

    def phase_B(self, l, bias_in):
        ar, ps, S = self.ar, self.ps, self.scr
        kT = ar.alloc("kT", [128, 6, T], BF16)
        self.memset("pool", kT[:], 0.0)
        qT = ar.alloc("qT", [128, 3, T], BF16)
        vtok = ar.alloc("vtok", [128, NT, 6, 65], BF16)
        bt = [ar.alloc(f"bt{i}", [128, 512], F32) for i in range(4)]
        sb = [ar.alloc(f"sb{i}", [128, 512], F32) for i in range(3)]
        pT = [ar.alloc(f"pT{i}", [128, 512], BF16) for i in range(4)]
        rec = [ar.alloc(f"rec{i}", [128, 6], F32) for i in range(2)]
        ytok = [ar.alloc(f"ytok{i}", [128, 384], BF16) for i in range(2)]
        yst = [ar.alloc(f"yst{i}", [128, 3, 128], BF16) for i in range(2)]
        c = dict(s=0, b=0, p=0, y=0)
        for b in range(self.nb_run):
            for h_ in range(6):
                po_ = (h_ % 2) * 64
                self.dma("sp", kT.v(kT.t[po_:po_ + 64, h_, :], key=h_), S["kB"][b].v(S["kB"][b].t[h_ // 2, po_:po_ + 64, :]))
            self.dma("sp", qT[:], S["qB"][b].v(S["qB"][b].t[:].rearrange("c p t -> p c t")))
            for t4 in range(0, NT, 4):
                te = min(NT, t4 + 4)
                self.dma("sp", vtok.v(vtok.t[:, t4:te], key=t4),
                         S["vB"][b].v(S["vB"][b].t[t4 * 128:te * 128].rearrange("(t p) h d -> p t h d", p=128)))
            qblocks = []
            if l < DEPTH - 1:
                qblocks.append((0, 256, [(0, None), (1, None)]))
            for qb in range(8):
                if qb == 0:
                    kl = [(2 + kb, kb) for kb in range(6)]
                elif qb == 7:
                    kl = [(2 + kb, 14 + kb - 26) for kb in range(26, 32)]
                else:
                    kl = [(2 + 4 * qb - 2 + j, 6 + j) for j in range(8)]
                qblocks.append((CTX + qb * 512, 512, kl + [(0, None), (1, None)]))
            for (q0, nq, klist) in qblocks:
                nqs = nq // 128
                tiles = [(h, ki, tau, bidx) for h in range(6) for ki, (tau, bidx) in enumerate(klist)]
                LOOK = 2
                pend = {}

                def issue_s(i):
                    h, ki, tau, bidx = tiles[i]
                    hp, po = h // 2, (h % 2) * 64
                    pst = ps[c["s"] % 3]; c["s"] += 1
                    self.mm(pst.v(pst.t[:, :nq]), kT.v(kT.t[:, h, tau * 128:(tau + 1) * 128]),
                            qT.v(qT.t[:, hp, q0:q0 + nq]))
                    p = pT[c["p"] % 4]; c["p"] += 1
                    if bidx is not None:
                        btile = bt[c["b"] % 4]; sbt = sb[c["b"] % 3]; c["b"] += 1
                        self.dma("sp", btile[:], bias_in.v(bias_in.t[l, h, bidx]))
                        self.tt("dve", sbt[:], pst[:], btile[:], ALU.add)
                        self.act(p[:], sbt[:], AF.Exp)
                    else:
                        self.act(p.v(p.t[:, :nq]), pst.v(pst.t[:, :nq]), AF.Exp)
                    pend[i] = p
                for i in range(min(LOOK, len(tiles))):
                    issue_s(i)
                for i, (h, ki, tau, bidx) in enumerate(tiles):
                    if i + LOOK < len(tiles):
                        issue_s(i + LOOK)
                    p = pend.pop(i)
                    for qs in range(nqs):
                        acc = ps[3 + qs]
                        self.mm(acc.v(acc.t[:, h * 65:(h + 1) * 65]), p.v(p.t[:, qs * 128:(qs + 1) * 128]),
                                vtok.v(vtok.t[:, tau, h, :]), start=(ki == 0), stop=(ki == len(klist) - 1))
                for qs in range(nqs):
                    acc = ps[3 + qs]
                    a3 = acc.t[:, 0:390].rearrange("p (h d) -> p h d", d=65)
                    rc = rec[c["y"] % 2]; yt = ytok[c["y"] % 2]; ys = yst[c["y"] % 2]; c["y"] += 1
                    self.recip(rc[:], acc.v(a3[:, :, 64]))
                    for h in range(6):
                        self.ts("dve", yt.v(yt.t[:, h * 64:(h + 1) * 64]), acc.v(a3[:, h, 0:64]), rc.v(rc.t[:, h:h + 1]), ALU.mult)
                    pt = ps[7]
                    for hp in range(3):
                        self.mm(pt.v(pt.t[:, hp * 128:(hp + 1) * 128]), yt.v(yt.t[:, hp * 128:(hp + 1) * 128]), self.identB[:])
                    self.cp("act", ys[:], pt.v(pt.t[:, 0:384].rearrange("p (c t) -> p c t", c=3)))
                    t0 = q0 + qs * 128
                    yTd = S["yT"][b]
                    self.dma("sp", yTd.v(yTd.t[2:5, :, t0:t0 + 128].rearrange("c p t -> p c t"), key=("b", t0)), ys[:])

    def phase_C(self, l, cst):
        ar, ps, S, P = self.ar, self.ps, self.scr, self.P
        fwd_order = list(range(NT))
        bwd_order = [1, 0] + list(range(NT - 1, 1, -1))
        identF, identB = self.identF, self.identB
        m0 = ar.mark()
        antiF = ar.alloc("antiF", [128, 128], F32); self.dma("sp", antiF[:], cst["antiF"][:])
        maskf = ar.alloc("maskf", [128, 128], F32); self.dma("sp", maskf[:], cst["maskf"][:])
        maskb = ar.alloc("maskb", [128, 128], F32); self.dma("sp", maskb[:], cst["maskb"][:])
        sel = ar.alloc("sel", [128, 12, 128], F32); self.dma("sp", sel[:], cst["sel"][:])
        mk = ar.alloc("mk", [12, 2], F32); self.dma("sp", mk[:], cst["mk"][:])
        gb = ar.alloc("gb", [12, DEPTH, 2], F32); self.dma("sp", gb[:], cst["gb"][:])
        ngb = ar.alloc("ngb", [12, 1], F32)
        self.ts("dve", ngb[:], gb.v(gb.t[:, l, 1:2]), -1.0, ALU.mult)
        rotT = ar.alloc("rotT", [128, 128], BF16); self.dma("pool", rotT[:], cst["rotT"][:])
        cw = ar.alloc("cw", [128, DEPTH, 2, 3, 3], F32); self.dma("sp", cw[:], cst["convW"][:])
        cb = ar.alloc("cb", [128, DEPTH, 2, 3], F32); self.dma("sp", cb[:], cst["convB"][:])
        Wtok = ar.alloc("Wtok", [128, NT, 3, 12], F32)
        dcol = ar.alloc("dcol", [128, 12, NT], F32)
        dcol2 = ar.alloc("dcol2", [128, 2, NT, 3, 1], F32)
        m1 = ar.mark()
        for b in range(self.nb_run):
            P.barrier(); ar.reset(m1)
            gtok = ar.alloc("gtok", [128, NT, 24], F32)
            for t4 in range(0, NT, 4):
                te = min(NT, t4 + 4)
                self.dma("sp", gtok.v(gtok.t[:, t4:te], key=t4),
                         S["gC"][b].v(S["gC"][b].t[t4 * 128:te * 128].rearrange("(t p) g -> p t g", p=128)))
            I12 = ar.alloc("I12", [12, T], F32); F12 = ar.alloc("F12", [12, T], F32)
            Fc = ar.alloc("Fc", [12, T], F32); Gm = ar.alloc("Gm", [12, T], F32)
            ones12 = ar.alloc("ones12", [12, T], F32)
            W3 = [ar.alloc(f"W3_{i}", [128, T], F32) for i in range(3)]
            self.memset("pool", ones12[:], 1.0)
            for w3 in W3:
                self.memset("pool", w3[:], 0.0)
            for g4 in range(0, NT, 4):
                nb = min(4, NT - g4)
                for typ, dst in ((0, I12), (1, F12)):
                    pa, pb_ = ps[(2 * typ) % 8], ps[(2 * typ + 1) % 8]
                    for i in range(nb):
                        pbk = g4 + i
                        self.mm(pa.v(pa.t[0:12, i * 128:(i + 1) * 128]), gtok.v(gtok.t[:, fwd_order[pbk], typ * 12:(typ + 1) * 12]), identF[:])
                        self.mm(pb_.v(pb_.t[0:12, i * 128:(i + 1) * 128]), gtok.v(gtok.t[:, bwd_order[pbk], typ * 12:(typ + 1) * 12]), antiF[:])
                    dv = dst.v(dst.t[:, g4 * 128:(g4 + nb) * 128])
                    self.ts("dve", dv, pa.v(pa.t[0:12, 0:nb * 128]), mk.v(mk.t[:, 0:1]), ALU.mult)
                    self.stt(dv, pb_.v(pb_.t[0:12, 0:nb * 128]), mk.v(mk.t[:, 1:2]), dv, ALU.mult, ALU.add)
            if self.upto == f"C2a_{l}":
                return
            self.act(F12[:], F12[:], AF.Exp, bias=ngb[:], scale=-1.0)
            self.act(F12[:], F12[:], AF.Ln, bias=1.0, scale=1.0)
            oa, d0, d1 = Fc.t[:], ones12.t[:], F12.t[:]
            P.op("dve", lambda e, oa=oa, d0=d0, d1=d1: e.tensor_tensor_scan(out=oa, data0=d0, data1=d1, initial=0.0,
                                                                          op0=ALU.mult, op1=ALU.subtract),
                 reads=[ones12[:], F12[:]], writes=[Fc[:]])
            self.stt(I12[:], I12[:], gb.v(gb.t[:, l, 0:1]), Fc[:], ALU.add, ALU.subtract)
            oa, d0 = Gm.t[:], I12.t[:]
            P.op("dve", lambda e, oa=oa, d0=d0: e.tensor_tensor_scan(out=oa, data0=d0, data1=d0, initial=0.0,
                                                                      op0=ALU.max, op1=ALU.max),
                 reads=[I12[:]], writes=[Gm[:]])
            if self.upto == f"C2b_{l}":
                return
            nGc = ar.alloc("nGc", [12, NT], F32); nGe = ar.alloc("nGe", [12, NT], F32); dch = ar.alloc("dch", [128, NT], F32)
            self.memset("pool", dch[:], 0.0)
            Gend = Gm.t[:].rearrange("p (c s) -> p c s", s=128)[:, :, 127]
            self.ts("dve", nGe[:], Gm.v(Gend), -1.0, ALU.mult)
            self.memset("dve", nGc.v(nGc.t[:, 0:1]), 0.0)
            self.cp("dve", nGc.v(nGc.t[:, 1:NT]), nGe.v(nGe.t[:, 0:NT - 1]))
            self.tt("dve", dch.v(dch.t[0:12, :]), nGe[:], nGc[:], ALU.subtract)
            self.act(dch.v(dch.t[0:12, :]), dch.v(dch.t[0:12, :]), AF.Exp)
            for c in range(NT):
                sl = slice(c * 128, (c + 1) * 128)
                self.act(W3[0].v(W3[0].t[0:12, sl]), I12.v(I12.t[:, sl]), AF.Exp, bias=nGc.v(nGc.t[:, c:c + 1]), scale=1.0)
                self.act(W3[1].v(W3[1].t[0:12, sl]), I12.v(I12.t[:, sl]), AF.Exp, bias=nGe.v(nGe.t[:, c:c + 1]), scale=1.0)
                self.act(W3[2].v(W3[2].t[0:12, sl]), Fc.v(Fc.t[:, sl]), AF.Exp, bias=nGc.v(nGc.t[:, c:c + 1]), scale=-1.0)
            if self.upto == f"C2c_{l}":
                return
            xs = [ar.alloc(f"xs{i}", [128, 36], F32) for i in range(2)]
            for pbk in range(NT):
                px = ps[pbk % 2]
                for kind in range(3):
                    self.mm(px.v(px.t[:, kind * 12:(kind + 1) * 12]), W3[kind].v(W3[kind].t[:, pbk * 128:(pbk + 1) * 128]),
                            identF.v(identF.t[:, 0:12]))
                x3 = px.t[:, 0:36].rearrange("p (k j) -> p k j", k=3)
                self.cp("dve", Wtok.v(Wtok.t[:, fwd_order[pbk], :, 0:6]), px.v(x3[:, :, 0:6]))
                xst = xs[pbk % 2]
                self.cp("act", xst[:], px.v(px.t[:, 0:36]))
                py = ps[2 + pbk % 2]
                self.mm(py.v(py.t[:, 0:36]), antiF[:], xst[:])
                y3 = py.t[:, 0:36].rearrange("p (k j) -> p k j", k=3)
                self.cp("dve", Wtok.v(Wtok.t[:, bwd_order[pbk], :, 6:12]), py.v(y3[:, :, 6:12]))
            if self.upto == f"C2d_{l}":
                return
            pd = ps[4]
            for j in range(12):
                self.mm(pd.v(pd.t[:, j * NT:(j + 1) * NT]), sel.v(sel.t[:, j, :]), dch[:])
            self.cp("dve", dcol[:], pd.v(pd.t[:, 0:12 * NT].rearrange("p (j c) -> p j c", j=12)))
            for d_ in range(2):
                for par in range(2):
                    for hp_ in range(3):
                        self.cp("dve", dcol2.v(dcol2.t[par * 64:(par + 1) * 64, d_, :, hp_, 0]),
                                dcol.v(dcol.t[par * 64:(par + 1) * 64, d_ * 6 + 2 * hp_ + par, :]))
            if f"Wtok{b}" in self.debug and l == 0:
                d = self.dram_scr(f"Wtok{b}", [128, NT * 36], F32)
                self.dma("sp", d[:], Wtok.v(Wtok.t[:].rearrange("p a b c -> p (a b c)")))
                d = self.dram_scr(f"dcol{b}", [128, 12 * NT], F32)
                self.dma("sp", d[:], dcol.v(dcol.t[:].rearrange("p a b -> p (a b)")))
            if self.upto == f"C2_{l}":
                return
            P.barrier(); ar.reset(m1)
            qT2 = ar.alloc("qT2", [128, 3, T], BF16)
            kT2 = ar.alloc("kT2", [128, 3, T], BF16)
            m2 = ar.mark()
            cosT = ar.alloc("cosT", [128, SEQ], F32); self.dma("sp", cosT[:], cst["cosT"][:])
            sinT = ar.alloc("sinT", [128, SEQ], F32); self.dma("sp", sinT[:], cst["sinT"][:])
            zin = [ar.alloc(f"zin{i}", [128, T], F32) for i in range(2)]
            acc = ar.alloc("cacc", [128, T], F32)
            sbf = ar.alloc("sbf", [128, T], BF16)
            t1 = [ar.alloc(f"t1_{i}", [128, 512], F32) for i in range(2)]
            t2 = [ar.alloc(f"t2_{i}", [128, 512], F32) for i in range(2)]
            it = 0
            for qk, (zsrc, dstT) in enumerate(((S["zqC"][b], qT2), (S["zkC"][b], kT2))):
                for c in range(3):
                    zi = zin[it % 2]; it += 1
                    self.dma("sp", zi[:], zsrc.v(zsrc.t[c]))
                    w = lambda tap: cw.v(cw.t[:, l, qk, c, tap:tap + 1])
                    self.act(acc[:], zi[:], AF.Identity, bias=cb.v(cb.t[:, l, qk, c:c + 1]), scale=w(1))
                    for (a0, a1) in ((0, CTX), (CTX, T)):
                        self.stt(acc.v(acc.t[:, a0 + 1:a1]), zi.v(zi.t[:, a0:a1 - 1]), w(0), acc.v(acc.t[:, a0 + 1:a1]), ALU.mult, ALU.add)
                        self.stt(acc.v(acc.t[:, a0:a1 - 1]), zi.v(zi.t[:, a0 + 1:a1]), w(2), acc.v(acc.t[:, a0:a1 - 1]), ALU.mult, ALU.add)
                    self.act(acc[:], acc[:], AF.Silu)
                    self.cp("pool", sbf[:], acc[:])
                    if qk == 0:
                        self.cp("pool", dstT.v(dstT.t[:, c, 0:CTX]), sbf.v(sbf.t[:, 0:CTX]))
                    else:
                        self.ts("pool", dstT.v(dstT.t[:, c, 0:CTX]), acc.v(acc.t[:, 0:CTX]), 0.125, ALU.mult)
                    for blk in range(8):
                        a0 = CTX + blk * 512
                        pr = ps[blk % 4]
                        self.mm(pr[:], rotT[:], sbf.v(sbf.t[:, a0:a0 + 512]))
                        ta, tb = t1[blk % 2], t2[blk % 2]
                        self.tt("dve", ta[:], acc.v(acc.t[:, a0:a0 + 512]), cosT.v(cosT.t[:, blk * 512:(blk + 1) * 512]), ALU.mult)
                        self.tt("dve", tb[:], pr[:], sinT.v(sinT.t[:, blk * 512:(blk + 1) * 512]), ALU.mult)
                        if qk == 0:
                            self.tt("dve", dstT.v(dstT.t[:, c, a0:a0 + 512]), ta[:], tb[:], ALU.add)
                        else:
                            self.tt("dve", ta[:], ta[:], tb[:], ALU.add)
                            self.act(dstT.v(dstT.t[:, c, a0:a0 + 512]), ta[:], AF.Copy, scale=0.125)
            if f"qT2_{b}" in self.debug and l == 0:
                d = self.dram_scr(f"qT2_{b}", [128, 3 * T], BF16)
                self.dma("sp", d[:], qT2.v(qT2.t[:].rearrange("p a b -> p (a b)")))
                d = self.dram_scr(f"kT2_{b}", [128, 3 * T], BF16)
                self.dma("sp", d[:], kT2.v(kT2.t[:].rearrange("p a b -> p (a b)")))
            if self.upto == f"C1_{l}":
                return
            P.barrier(); ar.reset(m2)
            ktok = ar.alloc("ktok", [128, NT, 384], BF16)
            v1 = ar.alloc("v1", [128, NT, 6, 65], BF16)
            for t4 in range(0, NT, 4):
                te = min(NT, t4 + 4)
                self.dma("sp", v1.v(v1.t[:, t4:te], key=t4),
                         S["vC"][b].v(S["vC"][b].t[t4 * 128:te * 128].rearrange("(t p) h d -> p t h d", p=128)))
            Hacc = ar.alloc("Hacc", [128, NT, 384], F32)
            for tau in range(NT):
                pk = ps[tau % 2]
                for c in range(3):
                    self.mm(pk.v(pk.t[:, c * 128:(c + 1) * 128]), kT2.v(kT2.t[:, c, tau * 128:(tau + 1) * 128]), identB[:])
                self.cp("act" if tau % 2 else "dve", ktok.v(ktok.t[:, tau, :]), pk.v(pk.t[:, 0:384]))
            S32 = [ar.alloc(f"S32_{d}", [128, 3, 65], F32) for d in range(2)]
            Sb = [ar.alloc(f"Sb_{d}", [128, 3, 65], BF16) for d in range(2)]
            for d in range(2):
                self.memset("pool", S32[d][:], 0.0); self.memset("pool", Sb[d][:], 0.0)
            smf = [ar.alloc(f"smf{i}", [128, 128], F32) for i in range(3)]
            smb = [ar.alloc(f"smb{i}", [128, 128], BF16) for i in range(4)]
            kw = [ar.alloc(f"kw{i}", [128, 64], BF16) for i in range(4)]
            dd = [ar.alloc(f"dd{i}", [128, 6, 1], F32) for i in range(2)]
            htmp = ar.alloc("htmp", [128, 6, 64], F32)
            cc = dict(a=0, s=0, k=0, n=0)
            hwritten = set()
            masks = (maskf, maskb)
            orders = (fwd_order, bwd_order)
            for c in range(NT):
                for d in range(2):
                    tau = orders[d][c]
                    tsl = slice(tau * 128, (tau + 1) * 128)
                    O = ps[4 + d]
                    U = ps[6 + d]
                    LOOK = 3
                    Abank = {}

                    def issue_a(h):
                        hp, po = h // 2, (h % 2) * 64
                        A = ps[cc["a"] % 4]; cc["a"] += 1
                        self.mm(A.v(A.t[:, 0:128]), kT2.v(kT2.t[po:po + 64, hp, tsl]), qT2.v(qT2.t[po:po + 64, hp, tsl]))
                        sm = smb[cc["s"] % 4]; cc["s"] += 1
                        self.stt(sm[:], A.v(A.t[:, 0:128]), Wtok.v(Wtok.t[:, tau, 0, d * 6 + h:d * 6 + h + 1]),
                                 masks[d][:], ALU.mult, ALU.mult)
                        kwt = kw[cc["k"] % 4]; cc["k"] += 1
                        self.act(kwt[:], ktok.v(ktok.t[:, tau, h * 64:(h + 1) * 64]), AF.Identity,
                                 scale=Wtok.v(Wtok.t[:, tau, 1, d * 6 + h:d * 6 + h + 1]))
                        Abank[h] = (sm, kwt)
                    for h in range(LOOK):
                        issue_a(h)
                    for h in range(6):
                        hp, po = h // 2, (h % 2) * 64
                        if h + LOOK < 6:
                            issue_a(h + LOOK)
                        sm, kwt = Abank.pop(h)
                        self.mm(O.v(O.t[:, h * 65:(h + 1) * 65]), sm[:], v1.v(v1.t[:, tau, h, :]), start=True, stop=False)
                        self.mm(O.v(O.t[:, h * 65:(h + 1) * 65]), qT2.v(qT2.t[po:po + 64, hp, tsl]),
                                Sb[d].v(Sb[d].t[po:po + 64, hp, :]), start=False, stop=True)
                        self.mm(U.v(U.t[po:po + 64, hp * 65:(hp + 1) * 65]), kwt[:], v1.v(v1.t[:, tau, h, :]))
                    O3 = O.t[:, 0:390].rearrange("p (h e) -> p h e", e=65)
                    ddt = dd[cc["n"] % 2]; cc["n"] += 1
                    self.act(ddt.v(ddt.t[:, :, 0]), O.v(O3[:, :, 64]), AF.Abs)
                    self.tt("dve", ddt.v(ddt.t[:, :, 0]), ddt.v(ddt.t[:, :, 0]), Wtok.v(Wtok.t[:, tau, 2, d * 6:(d + 1) * 6]), ALU.max)
                    self.recip(ddt[:], ddt[:])
                    rcb = ddt.v(ddt.t[:, :, 0:1].to_broadcast([128, 6, 64]))
                    first = tau not in hwritten
                    hwritten.add(tau)
                    hv3 = Hacc.v(Hacc.t[:, tau, :].rearrange("p (h e) -> p h e", e=64))
                    if first:
                        self.tt("dve", hv3, O.v(O3[:, :, 0:64]), rcb, ALU.mult)
                    else:
                        self.tt("dve", htmp[:], O.v(O3[:, :, 0:64]), rcb, ALU.mult)
                        self.tt("pool", hv3, hv3, htmp[:], ALU.add)
                    dcb = dcol2.v(dcol2.t[:, d, c, :, 0:1].to_broadcast([128, 3, 65]))
                    self.tt("dve", S32[d][:], S32[d][:], dcb, ALU.mult)
                    self.tt("dve", S32[d][:], S32[d][:], U.v(U.t[:, 0:195].rearrange("p (h e) -> p h e", e=65)), ALU.add)
                    self.cp("pool", Sb[d][:], S32[d][:])
            hb = [ar.alloc(f"hb{i}", [128, 384], BF16) for i in range(2)]
            ot = [ar.alloc(f"ot{i}", [128, 3, 128], BF16) for i in range(2)]
            yc = [ar.alloc(f"yc{i}", [128, 3, 128], BF16) for i in range(2)]
            for tau in range(NT):
                if l == DEPTH - 1 and tau < 2:
                    continue
                hbt, ott, yct = hb[tau % 2], ot[tau % 2], yc[tau % 2]
                tsl = slice(tau * 128, (tau + 1) * 128)
                self.dma("sp", ott[:], S["oC"][b].v(S["oC"][b].t[:, :, tsl].rearrange("c p t -> p c t")))
                self.cp("pool", hbt[:], Hacc.v(Hacc.t[:, tau, :]))
                pt = ps[tau % 4]
                for hp in range(3):
                    self.mm(pt.v(pt.t[:, hp * 128:(hp + 1) * 128]), hbt.v(hbt.t[:, hp * 128:(hp + 1) * 128]), identB[:])
                self.tt("dve", yct[:], pt.v(pt.t[:, 0:384].rearrange("p (c t) -> p c t", c=3)), ott[:], ALU.mult)
                yTd = S["yT"][b]
                self.dma("sp", yTd.v(yTd.t[5:8, :, tsl].rearrange("c p t -> p c t"), key=("c", tau)), yct[:])
        ar.reset(m0)

    def finish(self):
        P = self.P
        outs = [self.out_d[:]] + [d[:] for d in self.dbg_out.values()]
        P.fence("sp", outs)
        P.emit()
        return self.nc

    def phase_T(self, x_in, ctx_in, xT):
        ar, ps = self.ar, self.ps
        NBUF = 4
        xin = [ar.alloc(f"xin{i}", [128, D], F32) for i in range(NBUF)]
        xst = [ar.alloc(f"xst{i}", [128, NCH, 128], F32) for i in range(NBUF)]
        it = 0
        for b in range(self.nb_run):
            for tau in range(NT):
                xi, xs = xin[it % NBUF], xst[it % NBUF]
                if tau < 2:
                    src = ctx_in.v(ctx_in.t[b, tau * 128:(tau + 1) * 128, :])
                else:
                    src = x_in.v(x_in.t[b, (tau - 2) * 128:(tau - 1) * 128, :])
                self.dma("sp", xi[:], src)
                for half in range(2):
                    pst = ps[(it * 2 + half) % 8]
                    for jj in range(4):
                        j = half * 4 + jj
                        self.mm(pst.v(pst.t[:, jj * 128:(jj + 1) * 128]), xi.v(xi.t[:, j * 128:(j + 1) * 128]),
                                self.identF[:])
                    eng = "dve" if half == 0 else "act"
                    self.cp(eng, xs.v(xs.t[:, half * 4:(half + 1) * 4, :]),
                            pst.v(pst.t[:].rearrange("p (j t) -> p j t", j=4)))
                dst = xT[b].v(xT[b].t[:, :, tau * 128:(tau + 1) * 128].rearrange("j p t -> p j t"), key=tau)
                self.dma("sp", dst, xs[:])
                it += 1

    def norm_stats(self, xb, n, sqb, rstd):
        ps = self.ps
        pst = ps[7]
        for j in range(NCH):
            sq = sqb[j % 2]
            self.act(sq.v(sq.t[:, :n]), xb.v(xb.t[:, j, :n]), AF.Square)
            self.mm(pst.v(pst.t[:, :n]), self.onesB[:], sq.v(sq.t[:, :n]), start=(j == 0), stop=(j == NCH - 1))
        self.act(rstd.v(rstd.t[:, :n]), pst.v(pst.t[:, :n]), AF.Sqrt, bias=EPS, scale=1.0 / D)
        self.recip(rstd.v(rstd.t[:, :n]), rstd.v(rstd.t[:, :n]))

    def norm_apply(self, xb, n, l, nrm, src, xm, rstd, tmp, j):
        shift_kind = 0 if nrm == 0 else 3
        e1 = "dve" if j % 2 == 0 else "pool"
        tm = tmp[j % 2]
        self.tt(e1, tm.v(tm.t[:, :n]), xb.v(xb.t[:, j, :n]), rstd.v(rstd.t[:, :n]), ALU.mult)
        self.ts(e1, xm.v(xm.t[:, j, :n]), tm.v(tm.t[:, :n]),
                self.modA.v(self.modA.t[:, l, nrm, j, src:src + 1]), ALU.mult,
                self.modS.v(self.modS.t[:, l, shift_kind, j, src:src + 1]), ALU.add)

    def norm_mod(self, xb, n, l, nrm, src, xm, sqb, rstd, tmp):
        self.norm_stats(xb, n, sqb, rstd)
        for j in range(NCH):
            self.norm_apply(xb, n, l, nrm, src, xm, rstd, tmp, j)

    def phase_1(self, l, win_in, lng_in, lnb_in, wsT_in, bsB_in):
        ar, ps, P = self.ar, self.ps, self.P
        S = self.scr
        win = ar.alloc("win", [128, NCH, INC], BF16)
        for k in range(NCH):
            self.dma("pool", win.v(win.t[:, k, :], key=k), win_in.v(win_in.t[l, k * 128:(k + 1) * 128, :]),
                     max_dma_last_dim=4096)
        lng = ar.alloc("lng", [128, 256], F32); lnb = ar.alloc("lnb", [128, 256], F32)
        wsT = ar.alloc("wsT", [128, 4, 128], BF16); bsB = ar.alloc("bsB", [128, 2, 128], F32)
        self.dma("sp", lng[:], lng_in.v(lng_in.t[l])); self.dma("sp", lnb[:], lnb_in.v(lnb_in.t[l]))
        self.dma("pool", wsT[:], wsT_in.v(wsT_in.t[l])); self.dma("sp", bsB[:], bsB_in.v(bsB_in.t[l]))
        xb = [ar.alloc(f"xb{i}", [128, NCH, 512], F32) for i in range(2)]
        xm = [ar.alloc(f"xm{i}", [128, NCH, 512], BF16) for i in range(2)]
        sqb = [ar.alloc(f"sqb{i}", [128, 512], BF16) for i in range(2)]
        tmp = [ar.alloc(f"tmp{i}", [128, 512], F32) for i in range(2)]
        rstd = ar.alloc("rstd", [128, 512], F32)
        uT = [ar.alloc(f"uT{i}", [128, 2, 512], BF16) for i in range(2)]
        stB = [ar.alloc(f"stB{i}", [128, 512], BF16) for i in range(4)]
        stF = [ar.alloc(f"stF{i}", [128, 512], F32) for i in range(4)]
        vst = [ar.alloc(f"vst{i}", [128, 6, 65], BF16) for i in range(4)]
        for t_ in vst:
            self.memset("pool", t_[:], 1.0)
        gst = [ar.alloc(f"gst{i}", [128, 24], F32) for i in range(2)]
        av = [ar.alloc(f"av{i}", [128, 256], F32) for i in range(2)]
        av2 = ar.alloc("av2", [128, 256], F32)
        vln = [ar.alloc(f"vln{i}", [128, 256], BF16) for i in range(2)]
        st4 = ar.alloc("st4", [128, 16], F32)
        ya = [ar.alloc(f"ya{i}", [128, 2, 128], BF16) for i in range(2)]
        yaf = ar.alloc("yaf", [128, 2, 128], F32)
        cnt = dict(b=0, f=0, v=0, g=0, a=0, p=0)

        def nps():
            cnt["p"] += 1
            return ps[cnt["p"] % 6]

        it = 0
        items = [(b, s0, n) for b in range(self.nb_run) for (s0, n) in BLOCKS]

        def load(i):
            b_, s0_, n_ = items[i]
            xt_ = xb[i % 2]
            xTd_ = S["xT"][b_]
            self.dma("sp", xt_.v(xt_.t[:, :, :n_]), xTd_.v(xTd_.t[:, :, s0_:s0_ + n_].rearrange("j p t -> p j t")))
        load(0)
        self.norm_mod(xb[0], items[0][2], l, 0, (2 if items[0][1] < CTX else items[0][0]), xm[0], sqb, rstd, tmp)
        for (b, s0, n) in items:
            if True:
                src = 2 if s0 < CTX else b
                xbt, xmt, uTt = xb[it % 2], xm[it % 2], uT[it % 2]
                nxt = it + 1 < len(items)
                if nxt:
                    load(it + 1)
                    nb_, ns0_, nn_ = items[it + 1]
                    nsrc_ = 2 if ns0_ < CTX else nb_

                def fm(col0, nchunks, kind, dst):
                    for c in range(nchunks):
                        pst = nps()
                        for k in range(NCH):
                            self.mm(pst.v(pst.t[:, :n]), win.v(win.t[:, k, col0 + c * 128:col0 + (c + 1) * 128]),
                                    xmt.v(xmt.t[:, k, :n]), start=(k == 0), stop=(k == NCH - 1))
                        pv = pst.v(pst.t[:, :n])
                        if kind == "u":
                            self.act(uTt.v(uTt.t[:, c, :n]), pv, AF.Gelu)
                            continue
                        if kind in ("q", "k", "o"):
                            st = stB[cnt["b"] % 4]; cnt["b"] += 1
                            sv = st.v(st.t[:, :n])
                            if kind == "q":
                                self.act(sv, pv, AF.Copy, scale=0.125)
                            elif kind == "k":
                                self.cp("dve", sv, pv)
                            else:
                                self.act(sv, pv, AF.Sigmoid)
                        else:
                            st = stF[cnt["f"] % 4]; cnt["f"] += 1
                            sv = st.v(st.t[:, :n])
                            self.cp("dve", sv, pv)
                        self.dma("sp", dst[b].v(dst[b].t[c, :, s0:s0 + n], key=(c, s0)), sv)

                fm(O_AU, 2, "u", None)
                fm(O_BQ, 3, "q", S["qB"])
                fm(O_BK, 3, "k", S["kB"])
                fm(O_CQ, 3, "f", S["zqC"])
                fm(O_CK, 3, "f", S["zkC"])
                fm(O_CO, 3, "o", S["oC"])

                if nxt:
                    self.norm_stats(xb[(it + 1) % 2], nn_, sqb, rstd)
                nsub = n // 128
                per = NCH // nsub
                pending = []
                for sub in range(nsub):
                    t0 = s0 + sub * 128
                    lo = sub * 128

                    def tm(col0, ncols, pst):
                        for k in range(NCH):
                            self.mm(pst.v(pst.t[:, :ncols]), xmt.v(xmt.t[:, k, lo:lo + 128]),
                                    win.v(win.t[:, k, col0:col0 + ncols]), start=(k == 0), stop=(k == NCH - 1))
                        return pst.v(pst.t[:, :ncols])
                    for (col0, dst) in ((O_BV, S["vB"]), (O_CV, S["vC"])):
                        pv = tm(col0, 384, nps())
                        vs = vst[cnt["v"] % 4]; cnt["v"] += 1
                        self.cp("dve", vs.v(vs.t[:, :, 0:64]), VW(pv.ap.rearrange("p (h d) -> p h d", h=6), pv.b))
                        self.dma("sp", dst[b].v(dst[b].t[t0:t0 + 128], key=t0), vs[:])
                    pv = tm(O_CG, 24, nps())
                    gs = gst[cnt["g"] % 2]; cnt["g"] += 1
                    self.cp("dve", VW(gs.t[:].rearrange("p (b a c) -> p b a c", b=2, a=2), gs.b),
                            VW(pv.ap.rearrange("p (a b c) -> p b a c", a=2, b=2), pv.b))
                    self.dma("sp", S["gC"][b].v(S["gC"][b].t[t0:t0 + 128], key=t0), gs[:])
                    pv = tm(O_AV, 256, nps())
                    a1 = av[cnt["a"] % 2]; vl = vln[cnt["a"] % 2]; yat = ya[cnt["a"] % 2]; cnt["a"] += 1
                    self.act(a1[:], pv, AF.Gelu)
                    a3 = VW(a1.t[:].rearrange("p (g d) -> p g d", g=4), a1.b)
                    self.rsum(st4.v(st4.t[:, 0:4]), a3)
                    self.act(av2[:], a1[:], AF.Square)
                    self.rsum(st4.v(st4.t[:, 4:8]), VW(av2.t[:].rearrange("p (g d) -> p g d", g=4), av2.b))
                    self.ts("dve", st4.v(st4.t[:, 0:8]), st4.v(st4.t[:, 0:8]), 1.0 / 64, ALU.mult)
                    self.tt("dve", st4.v(st4.t[:, 8:12]), st4.v(st4.t[:, 0:4]), st4.v(st4.t[:, 0:4]), ALU.mult)
                    self.tt("dve", st4.v(st4.t[:, 12:16]), st4.v(st4.t[:, 4:8]), st4.v(st4.t[:, 8:12]), ALU.subtract)
                    self.act(st4.v(st4.t[:, 12:16]), st4.v(st4.t[:, 12:16]), AF.Sqrt, bias=EPS, scale=1.0)
                    self.recip(st4.v(st4.t[:, 12:16]), st4.v(st4.t[:, 12:16]))
                    for g in range(4):
                        self.ts("dve", av2.v(av2.t[:, g * 64:(g + 1) * 64]), a1.v(a1.t[:, g * 64:(g + 1) * 64]),
                                st4.v(st4.t[:, g:g + 1]), ALU.subtract, st4.v(st4.t[:, 12 + g:13 + g]), ALU.mult)
                    self.tt("pool", av2[:], av2[:], lng[:], ALU.mult)
                    self.tt("pool", vl[:], av2[:], lnb[:], ALU.add)
                    def mix(vl=vl, yat=yat, lo=lo, t0=t0, b=b, uTt=uTt):
                        pst = nps()
                        for g in range(4):
                            self.mm(pst.v(pst.t[(g % 2) * 64:(g % 2) * 64 + 64, (g // 2) * 128:(g // 2) * 128 + 128]),
                                    vl.v(vl.t[:, g * 64:(g + 1) * 64]), wsT.v(wsT.t[:, g, :]))
                        self.tt("dve", yaf[:], VW(pst.t[:, 0:256].rearrange("p (c t) -> p c t", c=2), pst.b), bsB[:], ALU.add)
                        self.tt("dve", yat[:], yaf[:], uTt.v(uTt.t[:, :, lo:lo + 128]), ALU.mult)
                        yTd = S["yT"][b]
                        self.dma("sp", yTd.v(yTd.t[0:2, :, t0:t0 + 128].rearrange("c p t -> p c t"), key=("a", t0)), yat[:])
                    for m_ in pending:
                        m_()
                    pending = [mix]
                    if nxt:
                        for j in range(sub * per, (sub + 1) * per):
                            self.norm_apply(xb[(it + 1) % 2], nn_, l, 0, nsrc_, xm[(it + 1) % 2], rstd, tmp, j)
                for m_ in pending:
                    m_()
                it += 1


def host_prep(inputs):
    f = lambda a: np.ascontiguousarray(np.asarray(a, dtype=np.float32))
    sh = {}
    sh["w_mod"] = f(inputs["w_mod"]); sh["w_in"] = f(inputs["w_in"]); sh["w_out"] = f(inputs["w_out"])
    sh["w_gate"] = f(inputs["w_gate"]); sh["w_up"] = f(inputs["w_up"]); sh["w_down"] = f(inputs["w_down"])
    sh["bmodT"] = f(np.asarray(inputs["b_mod"]).reshape(DEPTH, 6, NCH, 128).transpose(3, 0, 1, 2))
    sh["n1gT"] = f(np.asarray(inputs["norm1_g"]).reshape(DEPTH, NCH, 128).transpose(2, 0, 1))
    sh["n2gT"] = f(np.asarray(inputs["norm2_g"]).reshape(DEPTH, NCH, 128).transpose(2, 0, 1))
    sh["fgT"] = f(np.asarray(inputs["final_g"]).reshape(NCH, 128).transpose(1, 0))
    sh["lngB"] = f(np.broadcast_to(np.asarray(inputs["a_ln_g"]).reshape(DEPTH, 1, 256), (DEPTH, 128, 256)))
    sh["lnbB"] = f(np.broadcast_to(np.asarray(inputs["a_ln_b"]).reshape(DEPTH, 1, 256), (DEPTH, 128, 256)))
    sh["wsT"] = f(np.asarray(inputs["a_ws"]).transpose(0, 3, 1, 2))
    bs = np.asarray(inputs["a_bs"])
    bsB = np.broadcast_to(bs.reshape(DEPTH, 2, 2, 1, 128), (DEPTH, 2, 2, 64, 128))
    sh["bsB"] = f(bsB.transpose(0, 2, 3, 1, 4).reshape(DEPTH, 128, 2, 128))
    sh["identF"] = np.eye(128, dtype=np.float32)
    sh["biasT"] = build_bias(np.asarray(inputs["b_rpb"], dtype=np.float32))
    sh["antiF"] = np.ascontiguousarray(np.eye(128, dtype=np.float32)[::-1])
    ii = np.arange(128)
    sh["maskf"] = (ii[:, None] <= ii[None, :]).astype(np.float32)
    sh["maskb"] = (ii[:, None] >= ii[None, :]).astype(np.float32)
    sel = np.zeros((128, 12, 128), np.float32)
    for j in range(12):
        sel[j, j, :] = 1.0
    sh["sel"] = sel
    mk = np.zeros((12, 2), np.float32); mk[0:6, 0] = 1.0; mk[6:12, 1] = 1.0
    sh["mk"] = mk
    gbv = np.asarray(inputs["c_gate_b"], dtype=np.float32)
    gb = np.zeros((12, DEPTH, 2), np.float32)
    for l in range(DEPTH):
        gb[0:6, l, 0] = gbv[l, 0]; gb[6:12, l, 0] = gbv[l, 2]
        gb[0:6, l, 1] = gbv[l, 1]; gb[6:12, l, 1] = gbv[l, 3]
    sh["gb"] = gb
    rm = np.zeros((128, 128), np.float32)
    for p in range(128):
        if (p % 32) < 16:
            rm[p, p + 16] = -1.0
        else:
            rm[p, p - 16] = 1.0
    sh["rotT"] = np.ascontiguousarray(rm.T)
    cwv = np.asarray(inputs["c_conv_w"], dtype=np.float32)
    cbv = np.asarray(inputs["c_conv_b"], dtype=np.float32)
    sh["convW"] = f(cwv.reshape(DEPTH, 3, 2, 3, 128).transpose(4, 0, 2, 3, 1))
    sh["convB"] = f(cbv.reshape(DEPTH, 2, 3, 128).transpose(3, 0, 1, 2))
    inv_freq = (np.float32(10000.0) ** (-np.arange(16, dtype=np.float32) / np.float32(16))).astype(np.float32)
    t = np.arange(SEQ)
    pp = np.arange(128) % 64
    pos = np.where((pp // 32)[:, None] == 0, (t // GW)[None, :], (t % GW)[None, :]).astype(np.float32)
    ang = (pos * inv_freq[pp % 16][:, None]).astype(np.float32)
    sh["cosT"] = np.cos(ang).astype(np.float32)
    sh["sinT"] = np.sin(ang).astype(np.float32)
    return sh


def build_bias(rpb):
    out = np.full((DEPTH, 6, 20, 128, 512), NEG, np.float32)
    tiles = [(0, kb) for kb in range(6)] + [(1, 2 + j) for j in range(8)] + [(7, kb) for kb in range(26, 32)]
    for idx, (qb, kb) in enumerate(tiles):
        kr = np.repeat(2 * kb + np.arange(2), 64); kc = np.tile(np.arange(64), 2)
        qr = np.repeat(8 * qb + np.arange(8), 64); qc = np.tile(np.arange(64), 8)
        sr = np.clip(qr - 4, 0, 56); scc = np.clip(qc - 8, 0, 48)
        valid = ((kr[:, None] >= sr[None, :]) & (kr[:, None] < sr[None, :] + 8)
                 & (kc[:, None] >= scc[None, :]) & (kc[:, None] < scc[None, :] + 16))
        ri = np.clip(kr[:, None] - qr[None, :] + 7, 0, 14); ci = np.clip(kc[:, None] - qc[None, :] + 15, 0, 30)
        g = rpb[:, :, ri, ci]
        out[:, :, idx] = np.where(valid[None, None], g, np.float32(NEG))
    return out


def core_inputs(inputs, shared, c):
    m = dict(shared)
    b0 = c * NBL
    m["x"] = np.ascontiguousarray(np.asarray(inputs["x"], dtype=np.float32)[b0:b0 + NBL])
    m["ctx"] = np.ascontiguousarray(np.asarray(inputs["ctx"], dtype=np.float32)[b0:b0 + NBL])
    cv = np.stack([np.asarray(inputs["c"])[b0], np.asarray(inputs["c"])[b0 + 1], np.asarray(inputs["c_ctx"])], axis=0)
    m["cT"] = np.ascontiguousarray(cv.astype(np.float32).reshape(3, NCH, 128).transpose(2, 1, 0))
    return m


def kernel(**inputs):
    kb = K()
    nc = kb.build()
    shared = host_prep(inputs)
    in_maps = [core_inputs(inputs, shared, c) for c in range(8)]
    in_maps = [{k: v for k, v in m.items() if k in kb.din} for m in in_maps]
    res = run_bass_kernel_spmd(nc, in_maps, core_ids=list(range(8)))
    return np.concatenate([np.asarray(r["out"]) for r in res.results], axis=0).astype(np.float32)
```

```python
import bisect
import contextlib
import numpy as np
import concourse.bass as bass
import concourse.mybir as mybir
from concourse.bass_utils import run_bass_kernel_spmd

F32 = mybir.dt.float32
BF16 = mybir.dt.bfloat16
AF = mybir.ActivationFunctionType
ALU = mybir.AluOpType
AX = mybir.AxisListType

ENGINES = ("pe", "act", "dve", "pool", "sp")

D = 1024
NCH = 8
SEQ = 4096
CTX = 256
T = SEQ + CTX
NT = T // 128
DEPTH = 2
NBL = 2
DFF = 2816
NFF = DFF // 128
INC = 3224
EPS = 1e-6
GW = 64
NEG = -30000.0
O_AU, O_AV, O_BQ, O_BK, O_BV, O_CQ, O_CK, O_CV, O_CO, O_CG = 0, 256, 512, 896, 1280, 1664, 2048, 2432, 2816, 3200
BLOCKS = [(0, 256)] + [(256 + 512 * i, 512) for i in range(8)]


class Buf:
    __slots__ = ("name", "lastw", "readers", "sem", "semname", "psum")

    def __init__(self, name, semname=None):
        self.psum = False
        self.name = name
        self.semname = semname or name
        self.lastw = {}
        self.readers = {}
        self.sem = None


class Op:
    __slots__ = ("idx", "eng", "fn", "deps", "is_dma", "sem", "signal")

    def __init__(self, idx, eng, fn, is_dma):
        self.idx = idx; self.eng = eng; self.fn = fn; self.is_dma = is_dma
        self.deps = {}
        self.sem = None; self.signal = False


class VW:
    __slots__ = ("ap", "b", "key")

    def __init__(self, ap, b, key=None):
        self.ap = ap; self.b = b; self.key = key

    def k(self, key):
        return VW(self.ap, self.b, key)


class TL:
    def __init__(self, t, b):
        self.t = t; self.b = b

    def __getitem__(self, idx):
        return VW(self.t[idx], self.b)

    def v(self, ap, key=None):
        return VW(ap, self.b, key)


class Prog:
    def __init__(self, nc):
        self.nc = nc
        self.ops = []
        self.dma_sem_names = []
        self.nbuf = 0
        self.last_on = {e: None for e in ENGINES}
        self.dma_ops = []
        self.strict_war = False

    def buf(self, name=None, semname=None):
        self.nbuf += 1
        return Buf(name or f"b{self.nbuf}", semname)

    def _dep(self, op, idx, strong):
        if idx == op.idx:
            return
        op.deps[idx] = op.deps.get(idx, False) or strong

    def _track(self, op, reads, writes):
        for v in reads:
            b, k = v.b, v.key
            if b.psum:
                for rs in b.readers.values():
                    for r in rs:
                        if self.ops[r].eng != op.eng:
                            self._dep(op, r, True)
            if k is None:
                for w in b.lastw.values(): self._dep(op, w, True)
            else:
                if k in b.lastw: self._dep(op, b.lastw[k], True)
                if None in b.lastw: self._dep(op, b.lastw[None], True)
        for v in writes:
            b, k = v.b, v.key
            if k is None:
                for w in b.lastw.values(): self._dep(op, w, True)
                for rs in b.readers.values():
                    for r in rs: self._dep(op, r, False)
            else:
                for kk in (k, None):
                    if kk in b.lastw: self._dep(op, b.lastw[kk], True)
                    if kk in b.readers:
                        for r in b.readers[kk]: self._dep(op, r, False)
        for v in reads:
            v.b.readers.setdefault(v.key, []).append(op.idx)
        for v in writes:
            b, k = v.b, v.key
            if k is None:
                b.lastw = {None: op.idx}; b.readers = {}
            else:
                b.lastw[k] = op.idx; b.readers[k] = []

    def op(self, eng, fn, reads=(), writes=()):
        o = Op(len(self.ops), eng, fn, False)
        self.ops.append(o)
        self._track(o, reads, writes)
        self.last_on[eng] = o.idx
        return o

    def dma(self, queue, out, in_, **kw):
        o = Op(len(self.ops), queue, None, True)
        oap, iap = out.ap, in_.ap

        def fn(eng, oap=oap, iap=iap, kw=kw):
            return eng.dma_start(out=oap, in_=iap, **kw)
        o.fn = fn
        self.ops.append(o)
        wb = out.b
        if wb.sem is None:
            nm = "d_" + wb.semname
            if nm not in self.dma_sem_names:
                self.dma_sem_names.append(nm)
            wb.sem = self.dma_sem_names.index(nm)
        o.sem = wb.sem
        self._track(o, [in_], [out])
        self.dma_ops.append(o.idx)
        return o

    def fence(self, eng, reads):
        o = Op(len(self.ops), eng, None, False)
        self.ops.append(o)
        self._track(o, reads, ())
        return o

    def barrier(self):
        lasts = [i for i in self.last_on.values() if i is not None]
        dm = list(self.dma_ops)
        for e in ENGINES:
            o = Op(len(self.ops), e, None, False)
            self.ops.append(o)
            for i in lasts: self._dep(o, i, True)
            for i in dm: self._dep(o, i, True)
        self.dma_ops = []

    def emit(self):
        nc = self.nc
        ops = self.ops

        def needed(o, p, strong):
            if p.is_dma:
                return True
            if p.fn is None:
                return False
            if p.eng == o.eng and not o.is_dma:
                if o.eng == "pe":
                    return False
                return strong or self.strict_war
            return True

        red = {}
        for o in ops:
            best = {}
            dmas = {}
            for d, strong in o.deps.items():
                p = ops[d]
                if not needed(o, p, strong):
                    continue
                if p.is_dma:
                    dmas[p.sem] = p
                elif p.eng not in best or best[p.eng].idx < p.idx:
                    best[p.eng] = p
            red[o.idx] = list(best.values()) + list(dmas.values())
            for p in best.values():
                p.signal = True
        cnt = {e: 0 for e in ENGINES}
        sigval = {}
        EPOCH = 30000
        for o in ops:
            if o.is_dma or o.fn is None:
                continue
            if o.signal:
                cnt[o.eng] += 1
            c = cnt[o.eng]
            sigval[o.idx] = (0, 0) if c == 0 else ((c - 1) // EPOCH, (c - 1) % EPOCH + 1)
        nepoch = {e: max(1, (cnt[e] + EPOCH - 1) // EPOCH) for e in ENGINES}
        dma_lists = [[] for _ in self.dma_sem_names]
        for o in ops:
            if o.is_dma:
                dma_lists[o.sem].append(o.idx)
        self.stats = dict(n_ops=len(ops), sig=dict(cnt), n_dma_sems=len(self.dma_sem_names))
        with contextlib.ExitStack() as es:
            esems = {(e, k): es.enter_context(nc.semaphore(f"s_{e}{k}"))
                     for e in ("pe", "act", "dve", "pool") for k in range(nepoch[e])}
            dsems = [es.enter_context(nc.semaphore(n)) for n in self.dma_sem_names]
            block = es.enter_context(nc.Block())
            per_eng = {e: [o for o in ops if o.eng == e] for e in ENGINES}

            def run(engname, eng):
                seen = {}
                nwait = 0
                for o in per_eng[engname]:
                    waits = {}
                    for p in red[o.idx]:
                        if p.is_dma:
                            n = bisect.bisect_left(dma_lists[p.sem], o.idx)
                            key = ("d", p.sem); val = 16 * n
                        else:
                            ep, val = sigval[p.idx]
                            key = ("e", p.eng, ep)
                        if val > waits.get(key, 0):
                            waits[key] = val
                    for key, val in waits.items():
                        if seen.get(key, 0) >= val:
                            continue
                        seen[key] = val
                        sem = dsems[key[1]] if key[0] == "d" else esems[(key[1], key[2])]
                        eng.wait_ge(sem, val)
                        nwait += 1
                    if o.fn is None:
                        continue
                    ins = o.fn(eng)
                    if o.is_dma:
                        ins.then_inc(dsems[o.sem], 16)
                    elif o.signal:
                        ins.then_inc(esems[(engname, sigval[o.idx][0])], 1)
                self.stats["waits_" + engname] = nwait

            @block.tensor
            def _(eng): run("pe", eng)

            @block.scalar
            def _(eng): run("act", eng)

            @block.vector
            def _(eng): run("dve", eng)

            @block.gpsimd
            def _(eng): run("pool", eng)

            @block.sync
            def _(eng): run("sp", eng)


_DT_SIZE = {F32: 4, BF16: 2}


class Arena:
    def __init__(self, nc, P, base=17408, limit=212000):
        self.nc = nc; self.P = P; self.off = base; self.limit = limit; self.n = 0; self.peak = 0

    def alloc(self, name, shape, dtype):
        nbytes = int(np.prod(shape[1:])) * _DT_SIZE[dtype]
        self.off = (self.off + 63) // 64 * 64
        self.n += 1
        t = self.nc.alloc_sbuf_tensor_at(f"{name}_{self.n}", list(shape), dtype, offset=self.off)
        self.off += nbytes
        self.peak = max(self.peak, self.off)
        assert self.off <= self.limit, f"SBUF arena overflow at {name}: {self.off}"
        return TL(t, self.P.buf(f"{name}_{self.n}", semname=name))

    def mark(self):
        return self.off

    def reset(self, m):
        self.off = m


class K:
    def __init__(self, debug=None, upto=None):
        self.debug = debug or []
        self.upto = upto
        self.nb_run = 1 if "oneb" in self.debug else NBL
        nc = self.nc = bass.Bass("TRN2", target_bir_lowering=False)
        P = self.P = Prog(nc)
        self.ar = Arena(nc, P)
        self.din = {}
        self.dbg_out = {}

    def mm(self, out, lhsT, rhs, start=True, stop=True):
        oa, la, ra = out.ap, lhsT.ap, rhs.ap
        self.P.op("pe", lambda e: e.matmul(oa, lhsT=la, rhs=ra, start=start, stop=stop),
                  reads=[lhsT, rhs], writes=[out])

    def tr(self, out, in_, ident):
        oa, ia, ida = out.ap, in_.ap, ident.ap
        self.P.op("pe", lambda e: e.transpose(out=oa, in_=ia, identity=ida), reads=[in_, ident], writes=[out])

    def act(self, out, in_, func, bias=None, scale=None, eng="act", accum_out=None):
        oa, ia = out.ap, in_.ap
        kw = {}
        reads = [in_]
        if bias is not None:
            if isinstance(bias, VW):
                kw["bias"] = bias.ap; reads.append(bias)
            else:
                kw["bias"] = float(bias)
        if scale is not None:
            if isinstance(scale, VW):
                kw["scale"] = scale.ap; reads.append(scale)
            else:
                kw["scale"] = float(scale)
        writes = [out]
        if accum_out is not None:
            kw["accum_out"] = accum_out.ap; writes.append(accum_out)
        self.P.op("act", lambda e: e.activation(out=oa, in_=ia, func=func, **kw), reads=reads, writes=writes)

    def tt(self, eng, out, in0, in1, op):
        oa, a, b = out.ap, in0.ap, in1.ap
        self.P.op(eng, lambda e: e.tensor_tensor(out=oa, in0=a, in1=b, op=op), reads=[in0, in1], writes=[out])

    def ts(self, eng, out, in0, s1, op0, s2=None, op1=None):
        oa, a = out.ap, in0.ap
        reads = [in0]
        if isinstance(s1, VW):
            reads.append(s1); s1v = s1.ap
        else:
            s1v = float(s1)
        if isinstance(s2, VW):
            reads.append(s2); s2v = s2.ap
        else:
            s2v = None if s2 is None else float(s2)
        if op1 is None and eng == "pool" and op0 == ALU.mult:
            self.P.op(eng, lambda e: e.tensor_scalar(out=oa, in0=a, scalar1=s1v, scalar2=0.0, op0=ALU.mult, op1=ALU.add),
                      reads=reads, writes=[out])
        elif op1 is None:
            self.P.op(eng, lambda e: e.tensor_scalar(out=oa, in0=a, scalar1=s1v, scalar2=None, op0=op0),
                      reads=reads, writes=[out])
        else:
            self.P.op(eng, lambda e: e.tensor_scalar(out=oa, in0=a, scalar1=s1v, scalar2=s2v, op0=op0, op1=op1),
                      reads=reads, writes=[out])

    def stt(self, out, in0, scalar, in1, op0, op1):
        oa, a, b = out.ap, in0.ap, in1.ap
        reads = [in0, in1]
        if isinstance(scalar, VW):
            reads.append(scalar); sv = scalar.ap
        else:
            sv = float(scalar)
        self.P.op("dve", lambda e: e.scalar_tensor_tensor(out=oa, in0=a, scalar=sv, in1=b, op0=op0, op1=op1),
                  reads=reads, writes=[out])

    def cp(self, eng, out, in_):
        oa, ia = out.ap, in_.ap
        if eng == "act":
            self.P.op("act", lambda e: e.activation(out=oa, in_=ia, func=AF.Copy), reads=[in_], writes=[out])
        else:
            self.P.op(eng, lambda e: e.tensor_copy(out=oa, in_=ia), reads=[in_], writes=[out])

    def memset(self, eng, out, val):
        oa = out.ap
        self.P.op(eng, lambda e: e.memset(oa, float(val)), reads=[], writes=[out])

    def recip(self, out, in_):
        oa, ia = out.ap, in_.ap
        self.P.op("dve", lambda e: e.reciprocal(out=oa, in_=ia), reads=[in_], writes=[out])

    def rsum(self, out, in_, axis=AX.X):
        oa, ia = out.ap, in_.ap
        self.P.op("dve", lambda e: e.reduce_sum(out=oa, in_=ia, axis=axis), reads=[in_], writes=[out])

    def dma(self, q, out, in_, **kw):
        self.P.dma(q, out, in_, **kw)

    def dram_in(self, name, shape, dtype=F32):
        t = self.nc.dram_tensor(name, list(shape), dtype, kind="ExternalInput")
        tl = TL(t.ap(), self.P.buf(name))
        self.din[name] = tl
        return tl

    def dram_scr(self, name, shape, dtype):
        kind = "ExternalOutput" if name in self.debug else "Internal"
        t = self.nc.dram_tensor(name, list(shape), dtype, kind=kind)
        tl = TL(t.ap(), self.P.buf(name))
        if name in self.debug:
            self.dbg_out[name] = tl
        return tl

    def build(self):
        nc, P, ar = self.nc, self.P, self.ar
        x_in = self.dram_in("x", [NBL, SEQ, D])
        ctx_in = self.dram_in("ctx", [NBL, CTX, D])
        cT_in = self.dram_in("cT", [128, NCH, 3])
        wmod_in = self.dram_in("w_mod", [DEPTH, D, 6 * D])
        bmod_in = self.dram_in("bmodT", [128, DEPTH, 6, NCH])
        n1g_in = self.dram_in("n1gT", [128, DEPTH, NCH])
        n2g_in = self.dram_in("n2gT", [128, DEPTH, NCH])
        fg_in = self.dram_in("fgT", [128, NCH])
        win_in = self.dram_in("w_in", [DEPTH, D, INC])
        lng_in = self.dram_in("lngB", [DEPTH, 128, 256])
        lnb_in = self.dram_in("lnbB", [DEPTH, 128, 256])
        wsT_in = self.dram_in("wsT", [DEPTH, 128, 4, 128])
        bsB_in = self.dram_in("bsB", [DEPTH, 128, 2, 128])
        wout_in = self.dram_in("w_out", [DEPTH, D, D])
        wg_in = self.dram_in("w_gate", [DEPTH, D, DFF])
        wu_in = self.dram_in("w_up", [DEPTH, D, DFF])
        wd_in = self.dram_in("w_down", [DEPTH, DFF, D])
        idf_in = self.dram_in("identF", [128, 128])
        bias_in = self.dram_in("biasT", [DEPTH, 6, 20, 128, 512])
        cst = dict(antiF=self.dram_in("antiF", [128, 128]), maskf=self.dram_in("maskf", [128, 128]),
                   maskb=self.dram_in("maskb", [128, 128]), sel=self.dram_in("sel", [128, 12, 128]),
                   mk=self.dram_in("mk", [12, 2]), gb=self.dram_in("gb", [12, DEPTH, 2]),
                   rotT=self.dram_in("rotT", [128, 128]), convW=self.dram_in("convW", [128, DEPTH, 2, 3, 3]),
                   convB=self.dram_in("convB", [128, DEPTH, 2, 3]), cosT=self.dram_in("cosT", [128, SEQ]),
                   sinT=self.dram_in("sinT", [128, SEQ]))
        out_t = nc.dram_tensor("out", [NBL, SEQ, D], F32, kind="ExternalOutput")
        out_d = TL(out_t.ap(), P.buf("out"))
        self.out_d = out_d

        xT = [self.dram_scr(f"xT{b}", [NCH, 128, T], F32) for b in range(NBL)]
        yT = [self.dram_scr(f"yT{b}", [NCH, 128, T], BF16) for b in range(NBL)]
        qB = [self.dram_scr(f"qB{b}", [3, 128, T], BF16) for b in range(NBL)]
        kB = [self.dram_scr(f"kB{b}", [3, 128, T], BF16) for b in range(NBL)]
        vB = [self.dram_scr(f"vB{b}", [T, 6, 65], BF16) for b in range(NBL)]
        zqC = [self.dram_scr(f"zqC{b}", [3, 128, T], F32) for b in range(NBL)]
        zkC = [self.dram_scr(f"zkC{b}", [3, 128, T], F32) for b in range(NBL)]
        vC = [self.dram_scr(f"vC{b}", [T, 6, 65], BF16) for b in range(NBL)]
        oC = [self.dram_scr(f"oC{b}", [3, 128, T], BF16) for b in range(NBL)]
        gC = [self.dram_scr(f"gC{b}", [T, 24], F32) for b in range(NBL)]
        self.scr = dict(xT=xT, yT=yT, qB=qB, kB=kB, vB=vB, zqC=zqC, zkC=zkC, vC=vC, oC=oC, gC=gC)

        ps = [TL(nc.alloc_psum_tensor(f"ps{i}", [128, 512], F32), P.buf(f"ps{i}")) for i in range(8)]
        for p_ in ps:
            p_.b.psum = True
        self.ps = ps

        identF = ar.alloc("identF", [128, 128], F32)
        identB = ar.alloc("identB", [128, 128], BF16)
        onesB = ar.alloc("onesB", [128, 128], BF16)
        self.dma("sp", identF[:], idf_in[:])
        self.cp("dve", identB[:], identF[:])
        self.memset("dve", onesB[:], 1.0)
        self.identF, self.identB, self.onesB = identF, identB, onesB
        modS = ar.alloc("modS", [128, DEPTH, 6, NCH, 3], F32)
        modA = ar.alloc("modA", [128, DEPTH, 2, NCH, 3], F32)
        n1g = ar.alloc("n1g", [128, DEPTH, NCH], F32)
        n2g = ar.alloc("n2g", [128, DEPTH, NCH], F32)
        fgT = ar.alloc("fgT", [128, NCH], F32)
        bmodT = ar.alloc("bmodT", [128, DEPTH, 6, NCH], F32)
        self.dma("sp", n1g[:], n1g_in[:]); self.dma("sp", n2g[:], n2g_in[:])
        self.dma("sp", fgT[:], fg_in[:]); self.dma("sp", bmodT[:], bmod_in[:])
        self.modS, self.modA, self.fgT = modS, modA, fgT
        base_mark = ar.mark()

        if "skip01" in self.debug:
            for l in range(DEPTH):
                P.barrier(); ar.reset(base_mark)
                if "skipB" not in self.debug:
                    self.phase_B(l, bias_in)
                    if self.upto == f"B_{l}":
                        return self.finish()
                P.barrier(); ar.reset(base_mark)
                self.phase_C(l, cst)
                if self.upto in (f"C_{l}", f"C2_{l}", f"C1_{l}", f"C2a_{l}", f"C2b_{l}", f"C2c_{l}", f"C2d_{l}"):
                    return self.finish()
        scT = ar.alloc("scT", [128, NCH, 3], F32)
        self.dma("sp", scT[:], cT_in[:])
        self.act(scT[:], scT[:], AF.Silu)
        wm = [ar.alloc(f"wm{i}", [128, NCH, D], F32) for i in range(2)]
        it = 0
        for l in range(DEPTH):
            for i in range(6):
                w = wm[it % 2]
                src = wmod_in.v(wmod_in.t[l, :, i * D:(i + 1) * D].rearrange("(k p) n -> p k n", p=128))
                self.dma("sp", w[:], src)
                pst = ps[it % 2]
                for j in range(NCH):
                    for k in range(NCH):
                        self.mm(pst.v(pst.t[:, j * 3:(j + 1) * 3]), w.v(w.t[:, k, j * 128:(j + 1) * 128]),
                                scT.v(scT.t[:, k, :]), start=(k == 0), stop=(k == NCH - 1))
                for j in range(NCH):
                    self.ts("dve", modS.v(modS.t[:, l, i, j, :]), pst.v(pst.t[:, j * 3:(j + 1) * 3]),
                            bmodT.v(bmodT.t[:, l, i, j:j + 1]), ALU.add)
                it += 1
            for nrm, (sci, gt) in enumerate([(1, n1g), (4, n2g)]):
                for j in range(NCH):
                    self.ts("dve", modA.v(modA.t[:, l, nrm, j, :]), modS.v(modS.t[:, l, sci, j, :]),
                            1.0, ALU.add, gt.v(gt.t[:, l, j:j + 1]), ALU.mult)
        if "modS" in self.debug:
            d = self.dram_scr("modS", [128, DEPTH * 6 * NCH * 3], F32)
            self.dma("sp", d[:], modS.v(modS.t[:].rearrange("p a b c d -> p (a b c d)")))
        P.barrier()
        ar.reset(base_mark)

        self.phase_T(x_in, ctx_in, xT)
        if self.upto == "T":
            return self.finish()
        for l in range(DEPTH):
            P.barrier(); ar.reset(base_mark)
            self.phase_1(l, win_in, lng_in, lnb_in, wsT_in, bsB_in)
            if self.upto == f"1_{l}":
                return self.finish()
            P.barrier(); ar.reset(base_mark)
            if "skipB" not in self.debug:
                self.phase_B(l, bias_in)
            if self.upto == f"B_{l}":
                return self.finish()
            P.barrier(); ar.reset(base_mark)
            self.phase_C(l, cst)
            if self.upto in (f"C_{l}", f"C2_{l}", f"C1_{l}", f"C2a_{l}", f"C2b_{l}", f"C2c_{l}", f"C2d_{l}"):
                return self.finish()
            P.barrier(); ar.reset(base_mark)
            if f"inject_y{l}" in self.debug:
                yin = self.dram_in("yinj", [NBL, NCH, 128, T], BF16)
                for b in range(self.nb_run):
                    self.dma("sp", yT[b][:], yin.v(yin.t[b]))
                P.barrier()
            self.phase_4a(l, wout_in)
            P.barrier(); ar.reset(base_mark)
            self.phase_4b(l, wg_in, wu_in, wd_in)
            if self.upto == f"4_{l}":
                return self.finish()
        return self.finish()

    def phase_4a(self, l, wout_in):
        ar, ps, S = self.ar, self.ps, self.scr
        wo = ar.alloc("wo", [128, NCH, D], BF16)
        for k in range(NCH):
            self.dma("pool", wo.v(wo.t[:, k, :], key=k), wout_in.v(wout_in.t[l, k * 128:(k + 1) * 128, :]),
                     max_dma_last_dim=4096)
        xb = [ar.alloc(f"xb{i}", [128, NCH, 512], F32) for i in range(2)]
        yb = [ar.alloc(f"yb{i}", [128, NCH, 512], BF16) for i in range(2)]
        it = 0
        items = [(b, s0, n) for b in range(self.nb_run) for (s0, n) in BLOCKS if not (l == DEPTH - 1 and s0 < CTX)]

        def load(i):
            b_, s0_, n_ = items[i]
            xt_, yt_ = xb[i % 2], yb[i % 2]
            xTd_, yTd_ = S["xT"][b_], S["yT"][b_]
            self.dma("sp", yt_.v(yt_.t[:, :, :n_]), yTd_.v(yTd_.t[:, :, s0_:s0_ + n_].rearrange("j p t -> p j t")))
            self.dma("sp", xt_.v(xt_.t[:, :, :n_]), xTd_.v(xTd_.t[:, :, s0_:s0_ + n_].rearrange("j p t -> p j t")))
        load(0)
        for (b, s0, n) in items:
            if True:
                src = 2 if s0 < CTX else b
                xbt, ybt = xb[it % 2], yb[it % 2]
                xTd, yTd = S["xT"][b], S["yT"][b]
                if it + 1 < len(items):
                    load(it + 1)
                for j in range(NCH):
                    pst = ps[j % 4]
                    for k in range(NCH):
                        self.mm(pst.v(pst.t[:, :n]), wo.v(wo.t[:, k, j * 128:(j + 1) * 128]), ybt.v(ybt.t[:, k, :n]),
                                start=(k == 0), stop=(k == NCH - 1))
                    self.stt(xbt.v(xbt.t[:, j, :n]), pst.v(pst.t[:, :n]), self.modS.v(self.modS.t[:, l, 2, j, src:src + 1]),
                             xbt.v(xbt.t[:, j, :n]), ALU.mult, ALU.add)
                self.dma("sp", xTd.v(xTd.t[:, :, s0:s0 + n].rearrange("j p t -> p j t"), key=s0), xbt.v(xbt.t[:, :, :n]))
                it += 1

    def phase_4b(self, l, wg_in, wu_in, wd_in):
        ar, ps, S = self.ar, self.ps, self.scr
        last = (l == DEPTH - 1)
        NB = 256
        wg = ar.alloc("wg", [128, NCH, DFF], BF16)
        wu = ar.alloc("wu", [128, NCH, DFF], BF16)
        wd = ar.alloc("wd", [128, NFF, D], BF16)
        for k in range(NCH):
            self.dma("pool", wg.v(wg.t[:, k, :], key=k), wg_in.v(wg_in.t[l, k * 128:(k + 1) * 128, :]), max_dma_last_dim=4096)
            self.dma("pool", wu.v(wu.t[:, k, :], key=k), wu_in.v(wu_in.t[l, k * 128:(k + 1) * 128, :]), max_dma_last_dim=4096)
        for f in range(NFF):
            self.dma("pool", wd.v(wd.t[:, f, :], key=f), wd_in.v(wd_in.t[l, f * 128:(f + 1) * 128, :]), max_dma_last_dim=4096)
        xb = [ar.alloc(f"xb{i}", [128, NCH, NB], F32) for i in range(2)]
        xm = [ar.alloc(f"xm{i}", [128, NCH, NB], BF16) for i in range(2)]
        sqb = [ar.alloc(f"sqb{i}", [128, NB], BF16) for i in range(2)]
        tmp = [ar.alloc(f"tmp{i}", [128, NB], F32) for i in range(2)]
        rstd = [ar.alloc(f"rstd{i}", [128, NB], F32) for i in range(2)]
        rstf = ar.alloc("rstf", [128, NB], F32)
        sg = [ar.alloc(f"sg{i}", [128, NB], F32) for i in range(2)]
        h = ar.alloc("h", [128, NFF, NB], BF16)
        ost = [ar.alloc(f"ost{i}", [128, D], F32) for i in range(2)] if last else None
        n = NB
        items = [(b, s0) for b in range(self.nb_run) for s0 in range(0, T, NB) if not (last and s0 < CTX)]

        def load(i):
            b_, s0_ = items[i]
            xTd_ = S["xT"][b_]
            self.dma("sp", xb[i % 2][:], xTd_.v(xTd_.t[:, :, s0_:s0_ + n].rearrange("j p t -> p j t")))

        def srcof(i):
            b_, s0_ = items[i]
            return 2 if s0_ < CTX else b_
        oc = 0
        load(0)
        self.norm_mod(xb[0], n, l, 1, srcof(0), xm[0], sqb, rstd[0], tmp)
        for it, (b, s0) in enumerate(items):
            src = srcof(it)
            xbt, xmt = xb[it % 2], xm[it % 2]
            xTd = S["xT"][b]
            nxt = it + 1 < len(items)
            if nxt:
                load(it + 1)
            for f in range(NFF):
                pg, pu = ps[(2 * f) % 6], ps[(2 * f + 1) % 6]
                for k in range(NCH):
                    self.mm(pg.v(pg.t[:, :n]), wg.v(wg.t[:, k, f * 128:(f + 1) * 128]), xmt.v(xmt.t[:, k, :]),
                            start=(k == 0), stop=(k == NCH - 1))
                for k in range(NCH):
                    self.mm(pu.v(pu.t[:, :n]), wu.v(wu.t[:, k, f * 128:(f + 1) * 128]), xmt.v(xmt.t[:, k, :]),
                            start=(k == 0), stop=(k == NCH - 1))
                sgt = sg[f % 2]
                self.act(sgt[:], pg.v(pg.t[:, :n]), AF.Silu)
                self.tt("dve", h.v(h.t[:, f, :]), pu.v(pu.t[:, :n]), sgt[:], ALU.mult)
            if nxt:
                self.norm_stats(xb[(it + 1) % 2], n, sqb, rstd[(it + 1) % 2])
            for j in range(NCH):
                pst = ps[j % 6]
                for f in range(NFF):
                    self.mm(pst.v(pst.t[:, :n]), wd.v(wd.t[:, f, j * 128:(j + 1) * 128]), h.v(h.t[:, f, :]),
                            start=(f == 0), stop=(f == NFF - 1))
                if nxt:
                    self.norm_apply(xb[(it + 1) % 2], n, l, 1, srcof(it + 1), xm[(it + 1) % 2], rstd[(it + 1) % 2], tmp, j)
                self.stt(xbt.v(xbt.t[:, j, :]), pst.v(pst.t[:, :n]), self.modS.v(self.modS.t[:, l, 5, j, src:src + 1]),
                         xbt.v(xbt.t[:, j, :]), ALU.mult, ALU.add)
            if not last:
                self.dma("sp", xTd.v(xTd.t[:, :, s0:s0 + n].rearrange("j p t -> p j t"), key=s0), xbt[:])
            else:
                self.norm_stats(xbt, n, sqb, rstf)
                for j in range(NCH):
                    self.stt(xbt.v(xbt.t[:, j, :]), xbt.v(xbt.t[:, j, :]), self.fgT.v(self.fgT.t[:, j:j + 1]),
                             rstf[:], ALU.mult, ALU.mult)
                for sub in range(n // 128):
                    o = ost[oc % 2]; oc += 1
                    for half in range(2):
                        pst = ps[6 if half == 0 else (oc % 6)]
                        for jj in range(4):
                            j = half * 4 + jj
                            self.tr(pst.v(pst.t[:, jj * 128:(jj + 1) * 128]),
                                    xbt.v(xbt.t[:, j, sub * 128:(sub + 1) * 128]), self.identF[:])
                        self.cp("act" if half == 0 else "dve", o.v(o.t[:, half * 512:(half + 1) * 512]), pst[:])
                    t0 = s0 - CTX + sub * 128
                    self.dma("sp", self.out_d.v(self.out_d.t[b, t0:t0 + 128, :], key=(b, t0)), o[:])

    def phase_B(self, l, bias_in):
        ar, ps, S = self.ar, self.ps, self.scr
        kT = ar.alloc("kT", [128, 6, T], BF16)
        self.memset("pool", kT[:], 0.0)
        qT = ar.alloc("qT", [128, 3, T], BF16)
        vtok = ar.alloc("vtok", [128, NT, 6, 65], BF16)
        bt = [ar.alloc(f"bt{i}", [128, 512], F32) for i in range(4)]
        sb = [ar.alloc(f"sb{i}", [128, 512], F32) for i in range(3)]
        pT = [ar.alloc(f"pT{i}", [128, 512], BF16) for i in range(4)]
        rec = [ar.alloc(f"rec{i}", [128, 6], F32) for i in range(2)]
        ytok = [ar.alloc(f"ytok{i}", [128, 384], BF16) for i in range(2)]
        yst = [ar.alloc(f"yst{i}", [128, 3, 128], BF16) for i in range(2)]
        c = dict(s=0, b=0, p=0, y=0)
        for b in range(self.nb_run):
            for h_ in range(6):
                po_ = (h_ % 2) * 64
                self.dma("sp", kT.v(kT.t[po_:po_ + 64, h_, :], key=h_), S["kB"][b].v(S["kB"][b].t[h_ // 2, po_:po_ + 64, :]))
            self.dma("sp", qT[:], S["qB"][b].v(S["qB"][b].t[:].rearrange("c p t -> p c t")))
            for t4 in range(0, NT, 4):
                te = min(NT, t4 + 4)
                self.dma("sp", vtok.v(vtok.t[:, t4:te], key=t4),
                         S["vB"][b].v(S["vB"][b].t[t4 * 128:te * 128].rearrange("(t p) h d -> p t h d", p=128)))
            qblocks = []
            if l < DEPTH - 1:
                qblocks.append((0, 256, [(0, None), (1, None)]))
            for qb in range(8):
                if qb == 0:
                    kl = [(2 + kb, kb) for kb in range(6)]
                elif qb == 7:
                    kl = [(2 + kb, 14 + kb - 26) for kb in range(26, 32)]
                else:
                    kl = [(2 + 4 * qb - 2 + j, 6 + j) for j in range(8)]
                qblocks.append((CTX + qb * 512, 512, kl + [(0, None), (1, None)]))
            for (q0, nq, klist) in qblocks:
                nqs = nq // 128
                tiles = [(h, ki, tau, bidx) for h in range(6) for ki, (tau, bidx) in enumerate(klist)]
                LOOK = 2
                pend = {}

                def issue_s(i):
                    h, ki, tau, bidx = tiles[i]
                    hp, po = h // 2, (h % 2) * 64
                    pst = ps[c["s"] % 3]; c["s"] += 1
                    self.mm(pst.v(pst.t[:, :nq]), kT.v(kT.t[:, h, tau * 128:(tau + 1) * 128]),
                            qT.v(qT.t[:, hp, q0:q0 + nq]))
                    p = pT[c["p"] % 4]; c["p"] += 1
                    if bidx is not None:
                        btile = bt[c["b"] % 4]; sbt = sb[c["b"] % 3]; c["b"] += 1
                        self.dma("sp", btile[:], bias_in.v(bias_in.t[l, h, bidx]))
                        self.tt("dve", sbt[:], pst[:], btile[:], ALU.add)
                        self.act(p[:], sbt[:], AF.Exp)
                    else:
                        self.act(p.v(p.t[:, :nq]), pst.v(pst.t[:, :nq]), AF.Exp)
                    pend[i] = p
                for i in range(min(LOOK, len(tiles))):
                    issue_s(i)
                for i, (h, ki, tau, bidx) in enumerate(tiles):
                    if i + LOOK < len(tiles):
                        issue_s(i + LOOK)
                    p = pend.pop(i)
                    for qs in range(nqs):
                        acc = ps[3 + qs]
                        self.mm(acc.v(acc.t[:, h * 65:(h + 1) * 65]), p.v(p.t[:, qs * 128:(qs + 1) * 128]),
                                vtok.v(vtok.t[:, tau, h, :]), start=(ki == 0), stop=(ki == len(klist) - 1))
                for qs in range(nqs):
                    acc = ps[3 + qs]
                    a3 = acc.t[:, 0:390].rearrange("p (h d) -> p h d", d=65)
                    rc = rec[c["y"] % 2]; yt = ytok[c["y"] % 2]; ys = yst[c["y"] % 2]; c["y"] += 1
                    self.recip(rc[:], acc.v(a3[:, :, 64]))
                    for h in range(6):
                        self.ts("dve", yt.v(yt.t[:, h * 64:(h + 1) * 64]), acc.v(a3[:, h, 0:64]), rc.v(rc.t[:, h:h + 1]), ALU.mult)
                    pt = ps[7]
                    for hp in range(3):
                        self.mm(pt.v(pt.t[:, hp * 128:(hp + 1) * 128]), yt.v(yt.t[:, hp * 128:(hp + 1) * 128]), self.identB[:])
                    self.cp("act", ys[:], pt.v(pt.t[:, 0:384].rearrange("p (c t) -> p c t", c=3)))
                    t0 = q0 + qs * 128
                    yTd = S["yT"][b]
                    self.dma("sp", yTd.v(yTd.t[2:5, :, t0:t0 + 128].rearrange("c p t -> p c t"), key=("b", t0)), ys[:])

    def phase_C(self, l, cst):
        ar, ps, S, P = self.ar, self.ps, self.scr, self.P
        fwd_order = list(range(NT))
        bwd_order = [1, 0] + list(range(NT - 1, 1, -1))
        identF, identB = self.identF, self.identB
        m0 = ar.mark()
        antiF = ar.alloc("antiF", [128, 128], F32); self.dma("sp", antiF[:], cst["antiF"][:])
        maskf = ar.alloc("maskf", [128, 128], F32); self.dma("sp", maskf[:], cst["maskf"][:])
        maskb = ar.alloc("maskb", [128, 128], F32); self.dma("sp", maskb[:], cst["maskb"][:])
        sel = ar.alloc("sel", [128, 12, 128], F32); self.dma("sp", sel[:], cst["sel"][:])
        mk = ar.alloc("mk", [12, 2], F32); self.dma("sp", mk[:], cst["mk"][:])
        gb = ar.alloc("gb", [12, DEPTH, 2], F32); self.dma("sp", gb[:], cst["gb"][:])
        ngb = ar.alloc("ngb", [12, 1], F32)
        self.ts("dve", ngb[:], gb.v(gb.t[:, l, 1:2]), -1.0, ALU.mult)
        rotT = ar.alloc("rotT", [128, 128], BF16); self.dma("pool", rotT[:], cst["rotT"][:])
        cw = ar.alloc("cw", [128, DEPTH, 2, 3, 3], F32); self.dma("sp", cw[:], cst["convW"][:])
        cb = ar.alloc("cb", [128, DEPTH, 2, 3], F32); self.dma("sp", cb[:], cst["convB"][:])
        Wtok = ar.alloc("Wtok", [128, NT, 3, 12], F32)
        dcol = ar.alloc("dcol", [128, 12, NT], F32)
        dcol2 = ar.alloc("dcol2", [128, 2, NT, 3, 1], F32)
        m1 = ar.mark()
        for b in range(self.nb_run):
            P.barrier(); ar.reset(m1)
            gtok = ar.alloc("gtok", [128, NT, 24], F32)
            for t4 in range(0, NT, 4):
                te = min(NT, t4 + 4)
                self.dma("sp", gtok.v(gtok.t[:, t4:te], key=t4),
                         S["gC"][b].v(S["gC"][b].t[t4 * 128:te * 128].rearrange("(t p) g -> p t g", p=128)))
            I12 = ar.alloc("I12", [12, T], F32); F12 = ar.alloc("F12", [12, T], F32)
            Fc = ar.alloc("Fc", [12, T], F32); Gm = ar.alloc("Gm", [12, T], F32)
            ones12 = ar.alloc("ones12", [12, T], F32)
            W3 = [ar.alloc(f"W3_{i}", [128, T], F32) for i in range(3)]
            self.memset("pool", ones12[:], 1.0)
            for w3 in W3:
                self.memset("pool", w3[:], 0.0)
            for g4 in range(0, NT, 4):
                nb = min(4, NT - g4)
                for typ, dst in ((0, I12), (1, F12)):
                    pa, pb_ = ps[(2 * typ) % 8], ps[(2 * typ + 1) % 8]
                    for i in range(nb):
                        pbk = g4 + i
                        self.mm(pa.v(pa.t[0:12, i * 128:(i + 1) * 128]), gtok.v(gtok.t[:, fwd_order[pbk], typ * 12:(typ + 1) * 12]), identF[:])
                        self.mm(pb_.v(pb_.t[0:12, i * 128:(i + 1) * 128]), gtok.v(gtok.t[:, bwd_order[pbk], typ * 12:(typ + 1) * 12]), antiF[:])
                    dv = dst.v(dst.t[:, g4 * 128:(g4 + nb) * 128])
                    self.ts("dve", dv, pa.v(pa.t[0:12, 0:nb * 128]), mk.v(mk.t[:, 0:1]), ALU.mult)
                    self.stt(dv, pb_.v(pb_.t[0:12, 0:nb * 128]), mk.v(mk.t[:, 1:2]), dv, ALU.mult, ALU.add)
            if self.upto == f"C2a_{l}":
                return
            self.act(F12[:], F12[:], AF.Exp, bias=ngb[:], scale=-1.0)
            self.act(F12[:], F12[:], AF.Ln, bias=1.0, scale=1.0)
            oa, d0, d1 = Fc.t[:], ones12.t[:], F12.t[:]
            P.op("dve", lambda e, oa=oa, d0=d0, d1=d1: e.tensor_tensor_scan(out=oa, data0=d0, data1=d1, initial=0.0,
                                                                          op0=ALU.mult, op1=ALU.subtract),
                 reads=[ones12[:], F12[:]], writes=[Fc[:]])
            self.stt(I12[:], I12[:], gb.v(gb.t[:, l, 0:1]), Fc[:], ALU.add, ALU.subtract)
            oa, d0 = Gm.t[:], I12.t[:]
            P.op("dve", lambda e, oa=oa, d0=d0: e.tensor_tensor_scan(out=oa, data0=d0, data1=d0, initial=0.0,
                                                                      op0=ALU.max, op1=ALU.max),
                 reads=[I12[:]], writes=[Gm[:]])
            if self.upto == f"C2b_{l}":
                return
            nGc = ar.alloc("nGc", [12, NT], F32); nGe = ar.alloc("nGe", [12, NT], F32); dch = ar.alloc("dch", [128, NT], F32)
            self.memset("pool", dch[:], 0.0)
            Gend = Gm.t[:].rearrange("p (c s) -> p c s", s=128)[:, :, 127]
            self.ts("dve", nGe[:], Gm.v(Gend), -1.0, ALU.mult)
            self.memset("dve", nGc.v(nGc.t[:, 0:1]), 0.0)
            self.cp("dve", nGc.v(nGc.t[:, 1:NT]), nGe.v(nGe.t[:, 0:NT - 1]))
            self.tt("dve", dch.v(dch.t[0:12, :]), nGe[:], nGc[:], ALU.subtract)
            self.act(dch.v(dch.t[0:12, :]), dch.v(dch.t[0:12, :]), AF.Exp)
            for c in range(NT):
                sl = slice(c * 128, (c + 1) * 128)
                self.act(W3[0].v(W3[0].t[0:12, sl]), I12.v(I12.t[:, sl]), AF.Exp, bias=nGc.v(nGc.t[:, c:c + 1]), scale=1.0)
                self.act(W3[1].v(W3[1].t[0:12, sl]), I12.v(I12.t[:, sl]), AF.Exp, bias=nGe.v(nGe.t[:, c:c + 1]), scale=1.0)
                self.act(W3[2].v(W3[2].t[0:12, sl]), Fc.v(Fc.t[:, sl]), AF.Exp, bias=nGc.v(nGc.t[:, c:c + 1]), scale=-1.0)
            if self.upto == f"C2c_{l}":
                return
            xs = [ar.alloc(f"xs{i}", [128, 36], F32) for i in range(2)]
            for pbk in range(NT):
                px = ps[pbk % 2]
                for kind in range(3):
                    self.mm(px.v(px.t[:, kind * 12:(kind + 1) * 12]), W3[kind].v(W3[kind].t[:, pbk * 128:(pbk + 1) * 128]),
                            identF.v(identF.t[:, 0:12]))
                x3 = px.t[:, 0:36].rearrange("p (k j) -> p k j", k=3)
                self.cp("dve", Wtok.v(Wtok.t[:, fwd_order[pbk], :, 0:6]), px.v(x3[:, :, 0:6]))
                xst = xs[pbk % 2]
                self.cp("act", xst[:], px.v(px.t[:, 0:36]))
                py = ps[2 + pbk % 2]
                self.mm(py.v(py.t[:, 0:36]), antiF[:], xst[:])
                y3 = py.t[:, 0:36].rearrange("p (k j) -> p k j", k=3)
                self.cp("dve", Wtok.v(Wtok.t[:, bwd_order[pbk], :, 6:12]), py.v(y3[:, :, 6:12]))
            if self.upto == f"C2d_{l}":
                return
            pd = ps[4]
            for j in range(12):
                self.mm(pd.v(pd.t[:, j * NT:(j + 1) * NT]), sel.v(sel.t[:, j, :]), dch[:])
            self.cp("dve", dcol[:], pd.v(pd.t[:, 0:12 * NT].rearrange("p (j c) -> p j c", j=12)))
            for d_ in range(2):
                for par in range(2):
                    for hp_ in range(3):
                        self.cp("dve", dcol2.v(dcol2.t[par * 64:(par + 1) * 64, d_, :, hp_, 0]),
                                dcol.v(dcol.t[par * 64:(par + 1) * 64, d_ * 6 + 2 * hp_ + par, :]))
            if f"Wtok{b}" in self.debug and l == 0:
                d = self.dram_scr(f"Wtok{b}", [128, NT * 36], F32)
                self.dma("sp", d[:], Wtok.v(Wtok.t[:].rearrange("p a b c -> p (a b c)")))
                d = self.dram_scr(f"dcol{b}", [128, 12 * NT], F32)
                self.dma("sp", d[:], dcol.v(dcol.t[:].rearrange("p a b -> p (a b)")))
            if self.upto == f"C2_{l}":
                return
            P.barrier(); ar.reset(m1)
            qT2 = ar.alloc("qT2", [128, 3, T], BF16)
            kT2 = ar.alloc("kT2", [128, 3, T], BF16)
            m2 = ar.mark()
            cosT = ar.alloc("cosT", [128, SEQ], F32); self.dma("sp", cosT[:], cst["cosT"][:])
            sinT = ar.alloc("sinT", [128, SEQ], F32); self.dma("sp", sinT[:], cst["sinT"][:])
            zin = [ar.alloc(f"zin{i}", [128, T], F32) for i in range(2)]
            acc = ar.alloc("cacc", [128, T], F32)
            sbf = ar.alloc("sbf", [128, T], BF16)
            t1 = [ar.alloc(f"t1_{i}", [128, 512], F32) for i in range(2)]
            t2 = [ar.alloc(f"t2_{i}", [128, 512], F32) for i in range(2)]
            it = 0
            for qk, (zsrc, dstT) in enumerate(((S["zqC"][b], qT2), (S["zkC"][b], kT2))):
                for c in range(3):
                    zi = zin[it % 2]; it += 1
                    self.dma("sp", zi[:], zsrc.v(zsrc.t[c]))
                    w = lambda tap: cw.v(cw.t[:, l, qk, c, tap:tap + 1])
                    self.act(acc[:], zi[:], AF.Identity, bias=cb.v(cb.t[:, l, qk, c:c + 1]), scale=w(1))
                    for (a0, a1) in ((0, CTX), (CTX, T)):
                        self.stt(acc.v(acc.t[:, a0 + 1:a1]), zi.v(zi.t[:, a0:a1 - 1]), w(0), acc.v(acc.t[:, a0 + 1:a1]), ALU.mult, ALU.add)
                        self.stt(acc.v(acc.t[:, a0:a1 - 1]), zi.v(zi.t[:, a0 + 1:a1]), w(2), acc.v(acc.t[:, a0:a1 - 1]), ALU.mult, ALU.add)
                    self.act(acc[:], acc[:], AF.Silu)
                    self.cp("pool", sbf[:], acc[:])
                    if qk == 0:
                        self.cp("pool", dstT.v(dstT.t[:, c, 0:CTX]), sbf.v(sbf.t[:, 0:CTX]))
                    else:
                        self.ts("pool", dstT.v(dstT.t[:, c, 0:CTX]), acc.v(acc.t[:, 0:CTX]), 0.125, ALU.mult)
                    for blk in range(8):
                        a0 = CTX + blk * 512
                        pr = ps[blk % 4]
                        self.mm(pr[:], rotT[:], sbf.v(sbf.t[:, a0:a0 + 512]))
                        ta, tb = t1[blk % 2], t2[blk % 2]
                        self.tt("dve", ta[:], acc.v(acc.t[:, a0:a0 + 512]), cosT.v(cosT.t[:, blk * 512:(blk + 1) * 512]), ALU.mult)
                        self.tt("dve", tb[:], pr[:], sinT.v(sinT.t[:, blk * 512:(blk + 1) * 512]), ALU.mult)
                        if qk == 0:
                            self.tt("dve", dstT.v(dstT.t[:, c, a0:a0 + 512]), ta[:], tb[:], ALU.add)
                        else:
                            self.tt("dve", ta[:], ta[:], tb[:], ALU.add)
                            self.act(dstT.v(dstT.t[:, c, a0:a0 + 512]), ta[:], AF.Copy, scale=0.125)
            if f"qT2_{b}" in self.debug and l == 0:
                d = self.dram_scr(f"qT2_{b}", [128, 3 * T], BF16)
                self.dma("sp", d[:], qT2.v(qT2.t[:].rearrange("p a b -> p (a b)")))
                d = self.dram_scr(f"kT2_{b}", [128, 3 * T], BF16)
                self.dma("sp", d[:], kT2.v(kT2.t[:].rearrange("p a b -> p (a b)")))
            if self.upto == f"C1_{l}":
                return
            P.barrier(); ar.reset(m2)
            ktok = ar.alloc("ktok", [128, NT, 384], BF16)
            v1 = ar.alloc("v1", [128, NT, 6, 65], BF16)
            for t4 in range(0, NT, 4):
                te = min(NT, t4 + 4)
                self.dma("sp", v1.v(v1.t[:, t4:te], key=t4),
                         S["vC"][b].v(S["vC"][b].t[t4 * 128:te * 128].rearrange("(t p) h d -> p t h d", p=128)))
            Hacc = ar.alloc("Hacc", [128, NT, 384], F32)
            for tau in range(NT):
                pk = ps[tau % 2]
                for c in range(3):
                    self.mm(pk.v(pk.t[:, c * 128:(c + 1) * 128]), kT2.v(kT2.t[:, c, tau * 128:(tau + 1) * 128]), identB[:])
                self.cp("act" if tau % 2 else "dve", ktok.v(ktok.t[:, tau, :]), pk.v(pk.t[:, 0:384]))
            S32 = [ar.alloc(f"S32_{d}", [128, 3, 65], F32) for d in range(2)]
            Sb = [ar.alloc(f"Sb_{d}", [128, 3, 65], BF16) for d in range(2)]
            for d in range(2):
                self.memset("pool", S32[d][:], 0.0); self.memset("pool", Sb[d][:], 0.0)
            smf = [ar.alloc(f"smf{i}", [128, 128], F32) for i in range(3)]
            smb = [ar.alloc(f"smb{i}", [128, 128], BF16) for i in range(4)]
            kw = [ar.alloc(f"kw{i}", [128, 64], BF16) for i in range(4)]
            dd = [ar.alloc(f"dd{i}", [128, 6, 1], F32) for i in range(2)]
            htmp = ar.alloc("htmp", [128, 6, 64], F32)
            cc = dict(a=0, s=0, k=0, n=0)
            hwritten = set()
            masks = (maskf, maskb)
            orders = (fwd_order, bwd_order)
            for c in range(NT):
                for d in range(2):
                    tau = orders[d][c]
                    tsl = slice(tau * 128, (tau + 1) * 128)
                    O = ps[4 + d]
                    U = ps[6 + d]
                    LOOK = 3
                    Abank = {}

                    def issue_a(h):
                        hp, po = h // 2, (h % 2) * 64
                        A = ps[cc["a"] % 4]; cc["a"] += 1
                        self.mm(A.v(A.t[:, 0:128]), kT2.v(kT2.t[po:po + 64, hp, tsl]), qT2.v(qT2.t[po:po + 64, hp, tsl]))
                        sm = smb[cc["s"] % 4]; cc["s"] += 1
                        self.stt(sm[:], A.v(A.t[:, 0:128]), Wtok.v(Wtok.t[:, tau, 0, d * 6 + h:d * 6 + h + 1]),
                                 masks[d][:], ALU.mult, ALU.mult)
                        kwt = kw[cc["k"] % 4]; cc["k"] += 1
                        self.act(kwt[:], ktok.v(ktok.t[:, tau, h * 64:(h + 1) * 64]), AF.Identity,
                                 scale=Wtok.v(Wtok.t[:, tau, 1, d * 6 + h:d * 6 + h + 1]))
                        Abank[h] = (sm, kwt)
                    for h in range(LOOK):
                        issue_a(h)
                    for h in range(6):
                        hp, po = h // 2, (h % 2) * 64
                        if h + LOOK < 6:
                            issue_a(h + LOOK)
                        sm, kwt = Abank.pop(h)
                        self.mm(O.v(O.t[:, h * 65:(h + 1) * 65]), sm[:], v1.v(v1.t[:, tau, h, :]), start=True, stop=False)
                        self.mm(O.v(O.t[:, h * 65:(h + 1) * 65]), qT2.v(qT2.t[po:po + 64, hp, tsl]),
                                Sb[d].v(Sb[d].t[po:po + 64, hp, :]), start=False, stop=True)
                        self.mm(U.v(U.t[po:po + 64, hp * 65:(hp + 1) * 65]), kwt[:], v1.v(v1.t[:, tau, h, :]))
                    O3 = O.t[:, 0:390].rearrange("p (h e) -> p h e", e=65)
                    ddt = dd[cc["n"] % 2]; cc["n"] += 1
                    self.act(ddt.v(ddt.t[:, :, 0]), O.v(O3[:, :, 64]), AF.Abs)
                    self.tt("dve", ddt.v(ddt.t[:, :, 0]), ddt.v(ddt.t[:, :, 0]), Wtok.v(Wtok.t[:, tau, 2, d * 6:(d + 1) * 6]), ALU.max)
                    self.recip(ddt[:], ddt[:])
                    rcb = ddt.v(ddt.t[:, :, 0:1].to_broadcast([128, 6, 64]))
                    first = tau not in hwritten
                    hwritten.add(tau)
                    hv3 = Hacc.v(Hacc.t[:, tau, :].rearrange("p (h e) -> p h e", e=64))
                    if first:
                        self.tt("dve", hv3, O.v(O3[:, :, 0:64]), rcb, ALU.mult)
                    else:
                        self.tt("dve", htmp[:], O.v(O3[:, :, 0:64]), rcb, ALU.mult)
                        self.tt("pool", hv3, hv3, htmp[:], ALU.add)
                    dcb = dcol2.v(dcol2.t[:, d, c, :, 0:1].to_broadcast([128, 3, 65]))
                    self.tt("dve", S32[d][:], S32[d][:], dcb, ALU.mult)
                    self.tt("dve", S32[d][:], S32[d][:], U.v(U.t[:, 0:195].rearrange("p (h e) -> p h e", e=65)), ALU.add)
                    self.cp("pool", Sb[d][:], S32[d][:])
            hb = [ar.alloc(f"hb{i}", [128, 384], BF16) for i in range(2)]
            ot = [ar.alloc(f"ot{i}", [128, 3, 128], BF16) for i in range(2)]
            yc = [ar.alloc(f"yc{i}", [128, 3, 128], BF16) for i in range(2)]
            for tau in range(NT):
                if l == DEPTH - 1 and tau < 2:
                    continue
                hbt, ott, yct = hb[tau % 2], ot[tau % 2], yc[tau % 2]
                tsl = slice(tau * 128, (tau + 1) * 128)
                self.dma("sp", ott[:], S["oC"][b].v(S["oC"][b].t[:, :, tsl].rearrange("c p t -> p c t")))
                self.cp("pool", hbt[:], Hacc.v(Hacc.t[:, tau, :]))
                pt = ps[tau % 4]
                for hp in range(3):
                    self.mm(pt.v(pt.t[:, hp * 128:(hp + 1) * 128]), hbt.v(hbt.t[:, hp * 128:(hp + 1) * 128]), identB[:])
                self.tt("dve", yct[:], pt.v(pt.t[:, 0:384].rearrange("p (c t) -> p c t", c=3)), ott[:], ALU.mult)
                yTd = S["yT"][b]
                self.dma("sp", yTd.v(yTd.t[5:8, :, tsl].rearrange("c p t -> p c t"), key=("c", tau)), yct[:])
        ar.reset(m0)

    def finish(self):
        P = self.P
        outs = [self.out_d[:]] + [d[:] for d in self.dbg_out.values()]
        P.fence("sp", outs)
        P.emit()
        return self.nc

    def phase_T(self, x_in, ctx_in, xT):
        ar, ps = self.ar, self.ps
        NBUF = 4
        xin = [ar.alloc(f"xin{i}", [128, D], F32) for i in range(NBUF)]
        xst = [ar.alloc(f"xst{i}", [128, NCH, 128], F32) for i in range(NBUF)]
        it = 0
        for b in range(self.nb_run):
            for tau in range(NT):
                xi, xs = xin[it % NBUF], xst[it % NBUF]
                if tau < 2:
                    src = ctx_in.v(ctx_in.t[b, tau * 128:(tau + 1) * 128, :])
                else:
                    src = x_in.v(x_in.t[b, (tau - 2) * 128:(tau - 1) * 128, :])
                self.dma("sp", xi[:], src)
                for half in range(2):
                    pst = ps[(it * 2 + half) % 8]
                    for jj in range(4):
                        j = half * 4 + jj
                        self.tr(pst.v(pst.t[:, jj * 128:(jj + 1) * 128]), xi.v(xi.t[:, j * 128:(j + 1) * 128]),
                                self.identF[:])
                    eng = "dve" if half == 0 else "act"
                    self.cp(eng, xs.v(xs.t[:, half * 4:(half + 1) * 4, :]),
                            pst.v(pst.t[:].rearrange("p (j t) -> p j t", j=4)))
                dst = xT[b].v(xT[b].t[:, :, tau * 128:(tau + 1) * 128].rearrange("j p t -> p j t"), key=tau)
                self.dma("sp", dst, xs[:])
                it += 1

    def norm_stats(self, xb, n, sqb, rstd):
        ps = self.ps
        pst = ps[7]
        for j in range(NCH):
            sq = sqb[j % 2]
            self.act(sq.v(sq.t[:, :n]), xb.v(xb.t[:, j, :n]), AF.Square)
            self.mm(pst.v(pst.t[:, :n]), self.onesB[:], sq.v(sq.t[:, :n]), start=(j == 0), stop=(j == NCH - 1))
        self.act(rstd.v(rstd.t[:, :n]), pst.v(pst.t[:, :n]), AF.Sqrt, bias=EPS, scale=1.0 / D)
        self.recip(rstd.v(rstd.t[:, :n]), rstd.v(rstd.t[:, :n]))

    def norm_apply(self, xb, n, l, nrm, src, xm, rstd, tmp, j):
        shift_kind = 0 if nrm == 0 else 3
        e1 = "dve" if j % 2 == 0 else "pool"
        tm = tmp[j % 2]
        self.tt(e1, tm.v(tm.t[:, :n]), xb.v(xb.t[:, j, :n]), rstd.v(rstd.t[:, :n]), ALU.mult)
        self.ts(e1, xm.v(xm.t[:, j, :n]), tm.v(tm.t[:, :n]),
                self.modA.v(self.modA.t[:, l, nrm, j, src:src + 1]), ALU.mult,
                self.modS.v(self.modS.t[:, l, shift_kind, j, src:src + 1]), ALU.add)

    def norm_mod(self, xb, n, l, nrm, src, xm, sqb, rstd, tmp):
        self.norm_stats(xb, n, sqb, rstd)
        for j in range(NCH):
            self.norm_apply(xb, n, l, nrm, src, xm, rstd, tmp, j)

    def phase_1(self, l, win_in, lng_in, lnb_in, wsT_in, bsB_in):
        ar, ps, P = self.ar, self.ps, self.P
        S = self.scr
        win = ar.alloc("win", [128, NCH, INC], BF16)
        for k in range(NCH):
            self.dma("pool", win.v(win.t[:, k, :], key=k), win_in.v(win_in.t[l, k * 128:(k + 1) * 128, :]),
                     max_dma_last_dim=4096)
        lng = ar.alloc("lng", [128, 256], F32); lnb = ar.alloc("lnb", [128, 256], F32)
        wsT = ar.alloc("wsT", [128, 4, 128], BF16); bsB = ar.alloc("bsB", [128, 2, 128], F32)
        self.dma("sp", lng[:], lng_in.v(lng_in.t[l])); self.dma("sp", lnb[:], lnb_in.v(lnb_in.t[l]))
        self.dma("pool", wsT[:], wsT_in.v(wsT_in.t[l])); self.dma("sp", bsB[:], bsB_in.v(bsB_in.t[l]))
        xb = [ar.alloc(f"xb{i}", [128, NCH, 512], F32) for i in range(2)]
        xm = [ar.alloc(f"xm{i}", [128, NCH, 512], BF16) for i in range(2)]
        sqb = [ar.alloc(f"sqb{i}", [128, 512], BF16) for i in range(2)]
        tmp = [ar.alloc(f"tmp{i}", [128, 512], F32) for i in range(2)]
        rstd = ar.alloc("rstd", [128, 512], F32)
        uT = [ar.alloc(f"uT{i}", [128, 2, 512], BF16) for i in range(2)]
        stB = [ar.alloc(f"stB{i}", [128, 512], BF16) for i in range(4)]
        stF = [ar.alloc(f"stF{i}", [128, 512], F32) for i in range(4)]
        vst = [ar.alloc(f"vst{i}", [128, 6, 65], BF16) for i in range(4)]
        for t_ in vst:
            self.memset("pool", t_[:], 1.0)
        gst = [ar.alloc(f"gst{i}", [128, 24], F32) for i in range(2)]
        av = [ar.alloc(f"av{i}", [128, 256], F32) for i in range(2)]
        av2 = ar.alloc("av2", [128, 256], F32)
        vln = [ar.alloc(f"vln{i}", [128, 256], BF16) for i in range(2)]
        st4 = ar.alloc("st4", [128, 16], F32)
        ya = [ar.alloc(f"ya{i}", [128, 2, 128], BF16) for i in range(2)]
        yaf = ar.alloc("yaf", [128, 2, 128], F32)
        cnt = dict(b=0, f=0, v=0, g=0, a=0, p=0)

        def nps():
            cnt["p"] += 1
            return ps[cnt["p"] % 6]

        it = 0
        items = [(b, s0, n) for b in range(self.nb_run) for (s0, n) in BLOCKS]

        def load(i):
            b_, s0_, n_ = items[i]
            xt_ = xb[i % 2]
            xTd_ = S["xT"][b_]
            self.dma("sp", xt_.v(xt_.t[:, :, :n_]), xTd_.v(xTd_.t[:, :, s0_:s0_ + n_].rearrange("j p t -> p j t")))
        load(0)
        self.norm_mod(xb[0], items[0][2], l, 0, (2 if items[0][1] < CTX else items[0][0]), xm[0], sqb, rstd, tmp)
        for (b, s0, n) in items:
            if True:
                src = 2 if s0 < CTX else b
                xbt, xmt, uTt = xb[it % 2], xm[it % 2], uT[it % 2]
                nxt = it + 1 < len(items)
                if nxt:
                    load(it + 1)
                    nb_, ns0_, nn_ = items[it + 1]
                    nsrc_ = 2 if ns0_ < CTX else nb_

                def fm(col0, nchunks, kind, dst):
                    for c in range(nchunks):
                        pst = nps()
                        for k in range(NCH):
                            self.mm(pst.v(pst.t[:, :n]), win.v(win.t[:, k, col0 + c * 128:col0 + (c + 1) * 128]),
                                    xmt.v(xmt.t[:, k, :n]), start=(k == 0), stop=(k == NCH - 1))
                        pv = pst.v(pst.t[:, :n])
                        if kind == "u":
                            self.act(uTt.v(uTt.t[:, c, :n]), pv, AF.Gelu)
                            continue
                        if kind in ("q", "k", "o"):
                            st = stB[cnt["b"] % 4]; cnt["b"] += 1
                            sv = st.v(st.t[:, :n])
                            if kind == "q":
                                self.act(sv, pv, AF.Copy, scale=0.125)
                            elif kind == "k":
                                self.cp("dve", sv, pv)
                            else:
                                self.act(sv, pv, AF.Sigmoid)
                        else:
                            st = stF[cnt["f"] % 4]; cnt["f"] += 1
                            sv = st.v(st.t[:, :n])
                            self.cp("dve", sv, pv)
                        self.dma("sp", dst[b].v(dst[b].t[c, :, s0:s0 + n], key=(c, s0)), sv)

                fm(O_AU, 2, "u", None)
                fm(O_BQ, 3, "q", S["qB"])
                fm(O_BK, 3, "k", S["kB"])
                fm(O_CQ, 3, "f", S["zqC"])
                fm(O_CK, 3, "f", S["zkC"])
                fm(O_CO, 3, "o", S["oC"])

                if nxt:
                    self.norm_stats(xb[(it + 1) % 2], nn_, sqb, rstd)
                nsub = n // 128
                per = NCH // nsub
                pending = []
                for sub in range(nsub):
                    t0 = s0 + sub * 128
                    lo = sub * 128

                    def tm(col0, ncols, pst):
                        for k in range(NCH):
                            self.mm(pst.v(pst.t[:, :ncols]), xmt.v(xmt.t[:, k, lo:lo + 128]),
                                    win.v(win.t[:, k, col0:col0 + ncols]), start=(k == 0), stop=(k == NCH - 1))
                        return pst.v(pst.t[:, :ncols])
                    for (col0, dst) in ((O_BV, S["vB"]), (O_CV, S["vC"])):
                        pv = tm(col0, 384, nps())
                        vs = vst[cnt["v"] % 4]; cnt["v"] += 1
                        self.cp("dve", vs.v(vs.t[:, :, 0:64]), VW(pv.ap.rearrange("p (h d) -> p h d", h=6), pv.b))
                        self.dma("sp", dst[b].v(dst[b].t[t0:t0 + 128], key=t0), vs[:])
                    pv = tm(O_CG, 24, nps())
                    gs = gst[cnt["g"] % 2]; cnt["g"] += 1
                    self.cp("dve", VW(gs.t[:].rearrange("p (b a c) -> p b a c", b=2, a=2), gs.b),
                            VW(pv.ap.rearrange("p (a b c) -> p b a c", a=2, b=2), pv.b))
                    self.dma("sp", S["gC"][b].v(S["gC"][b].t[t0:t0 + 128], key=t0), gs[:])
                    pv = tm(O_AV, 256, nps())
                    a1 = av[cnt["a"] % 2]; vl = vln[cnt["a"] % 2]; yat = ya[cnt["a"] % 2]; cnt["a"] += 1
                    self.act(a1[:], pv, AF.Gelu)
                    a3 = VW(a1.t[:].rearrange("p (g d) -> p g d", g=4), a1.b)
                    self.rsum(st4.v(st4.t[:, 0:4]), a3)
                    self.act(av2[:], a1[:], AF.Square)
                    self.rsum(st4.v(st4.t[:, 4:8]), VW(av2.t[:].rearrange("p (g d) -> p g d", g=4), av2.b))
                    self.ts("dve", st4.v(st4.t[:, 0:8]), st4.v(st4.t[:, 0:8]), 1.0 / 64, ALU.mult)
                    self.tt("dve", st4.v(st4.t[:, 8:12]), st4.v(st4.t[:, 0:4]), st4.v(st4.t[:, 0:4]), ALU.mult)
                    self.tt("dve", st4.v(st4.t[:, 12:16]), st4.v(st4.t[:, 4:8]), st4.v(st4.t[:, 8:12]), ALU.subtract)
                    self.act(st4.v(st4.t[:, 12:16]), st4.v(st4.t[:, 12:16]), AF.Sqrt, bias=EPS, scale=1.0)
                    self.recip(st4.v(st4.t[:, 12:16]), st4.v(st4.t[:, 12:16]))
                    for g in range(4):
                        self.ts("dve", av2.v(av2.t[:, g * 64:(g + 1) * 64]), a1.v(a1.t[:, g * 64:(g + 1) * 64]),
                                st4.v(st4.t[:, g:g + 1]), ALU.subtract, st4.v(st4.t[:, 12 + g:13 + g]), ALU.mult)
                    self.tt("pool", av2[:], av2[:], lng[:], ALU.mult)
                    self.tt("pool", vl[:], av2[:], lnb[:], ALU.add)
                    def mix(vl=vl, yat=yat, lo=lo, t0=t0, b=b, uTt=uTt):
                        pst = nps()
                        for g in range(4):
                            self.mm(pst.v(pst.t[(g % 2) * 64:(g % 2) * 64 + 64, (g // 2) * 128:(g // 2) * 128 + 128]),
                                    vl.v(vl.t[:, g * 64:(g + 1) * 64]), wsT.v(wsT.t[:, g, :]))
                        self.tt("dve", yaf[:], VW(pst.t[:, 0:256].rearrange("p (c t) -> p c t", c=2), pst.b), bsB[:], ALU.add)
                        self.tt("dve", yat[:], yaf[:], uTt.v(uTt.t[:, :, lo:lo + 128]), ALU.mult)
                        yTd = S["yT"][b]
                        self.dma("sp", yTd.v(yTd.t[0:2, :, t0:t0 + 128].rearrange("c p t -> p c t"), key=("a", t0)), yat[:])
                    for m_ in pending:
                        m_()
                    pending = [mix]
                    if nxt:
                        for j in range(sub * per, (sub + 1) * per):
                            self.norm_apply(xb[(it + 1) % 2], nn_, l, 0, nsrc_, xm[(it + 1) % 2], rstd, tmp, j)
                for m_ in pending:
                    m_()
                it += 1


def host_prep(inputs):
    f = lambda a: np.ascontiguousarray(np.asarray(a, dtype=np.float32))
    sh = {}
    sh["w_mod"] = f(inputs["w_mod"]); sh["w_in"] = f(inputs["w_in"]); sh["w_out"] = f(inputs["w_out"])
    sh["w_gate"] = f(inputs["w_gate"]); sh["w_up"] = f(inputs["w_up"]); sh["w_down"] = f(inputs["w_down"])
    sh["bmodT"] = f(np.asarray(inputs["b_mod"]).reshape(DEPTH, 6, NCH, 128).transpose(3, 0, 1, 2))
    sh["n1gT"] = f(np.asarray(inputs["norm1_g"]).reshape(DEPTH, NCH, 128).transpose(2, 0, 1))
    sh["n2gT"] = f(np.asarray(inputs["norm2_g"]).reshape(DEPTH, NCH, 128).transpose(2, 0, 1))
    sh["fgT"] = f(np.asarray(inputs["final_g"]).reshape(NCH, 128).transpose(1, 0))
    sh["lngB"] = f(np.broadcast_to(np.asarray(inputs["a_ln_g"]).reshape(DEPTH, 1, 256), (DEPTH, 128, 256)))
    sh["lnbB"] = f(np.broadcast_to(np.asarray(inputs["a_ln_b"]).reshape(DEPTH, 1, 256), (DEPTH, 128, 256)))
    sh["wsT"] = f(np.asarray(inputs["a_ws"]).transpose(0, 3, 1, 2))
    bs = np.asarray(inputs["a_bs"])
    bsB = np.broadcast_to(bs.reshape(DEPTH, 2, 2, 1, 128), (DEPTH, 2, 2, 64, 128))
    sh["bsB"] = f(bsB.transpose(0, 2, 3, 1, 4).reshape(DEPTH, 128, 2, 128))
    sh["identF"] = np.eye(128, dtype=np.float32)
    sh["biasT"] = build_bias(np.asarray(inputs["b_rpb"], dtype=np.float32))
    sh["antiF"] = np.ascontiguousarray(np.eye(128, dtype=np.float32)[::-1])
    ii = np.arange(128)
    sh["maskf"] = (ii[:, None] <= ii[None, :]).astype(np.float32)
    sh["maskb"] = (ii[:, None] >= ii[None, :]).astype(np.float32)
    sel = np.zeros((128, 12, 128), np.float32)
    for j in range(12):
        sel[j, j, :] = 1.0
    sh["sel"] = sel
    mk = np.zeros((12, 2), np.float32); mk[0:6, 0] = 1.0; mk[6:12, 1] = 1.0
    sh["mk"] = mk
    gbv = np.asarray(inputs["c_gate_b"], dtype=np.float32)
    gb = np.zeros((12, DEPTH, 2), np.float32)
    for l in range(DEPTH):
        gb[0:6, l, 0] = gbv[l, 0]; gb[6:12, l, 0] = gbv[l, 2]
        gb[0:6, l, 1] = gbv[l, 1]; gb[6:12, l, 1] = gbv[l, 3]
    sh["gb"] = gb
    rm = np.zeros((128, 128), np.float32)
    for p in range(128):
        if (p % 32) < 16:
            rm[p, p + 16] = -1.0
        else:
            rm[p, p - 16] = 1.0
    sh["rotT"] = np.ascontiguousarray(rm.T)
    cwv = np.asarray(inputs["c_conv_w"], dtype=np.float32)
    cbv = np.asarray(inputs["c_conv_b"], dtype=np.float32)
    sh["convW"] = f(cwv.reshape(DEPTH, 3, 2, 3, 128).transpose(4, 0, 2, 3, 1))
    sh["convB"] = f(cbv.reshape(DEPTH, 2, 3, 128).transpose(3, 0, 1, 2))
    inv_freq = (np.float32(10000.0) ** (-np.arange(16, dtype=np.float32) / np.float32(16))).astype(np.float32)
    t = np.arange(SEQ)
    pp = np.arange(128) % 64
    pos = np.where((pp // 32)[:, None] == 0, (t // GW)[None, :], (t % GW)[None, :]).astype(np.float32)
    ang = (pos * inv_freq[pp % 16][:, None]).astype(np.float32)
    sh["cosT"] = np.cos(ang).astype(np.float32)
    sh["sinT"] = np.sin(ang).astype(np.float32)
    return sh


def build_bias(rpb):
    out = np.full((DEPTH, 6, 20, 128, 512), NEG, np.float32)
    tiles = [(0, kb) for kb in range(6)] + [(1, 2 + j) for j in range(8)] + [(7, kb) for kb in range(26, 32)]
    for idx, (qb, kb) in enumerate(tiles):
        kr = np.repeat(2 * kb + np.arange(2), 64); kc = np.tile(np.arange(64), 2)
        qr = np.repeat(8 * qb + np.arange(8), 64); qc = np.tile(np.arange(64), 8)
        sr = np.clip(qr - 4, 0, 56); scc = np.clip(qc - 8, 0, 48)
        valid = ((kr[:, None] >= sr[None, :]) & (kr[:, None] < sr[None, :] + 8)
                 & (kc[:, None] >= scc[None, :]) & (kc[:, None] < scc[None, :] + 16))
        ri = np.clip(kr[:, None] - qr[None, :] + 7, 0, 14); ci = np.clip(kc[:, None] - qc[None, :] + 15, 0, 30)
        g = rpb[:, :, ri, ci]
        out[:, :, idx] = np.where(valid[None, None], g, np.float32(NEG))
    return out


def core_inputs(inputs, shared, c):
    m = dict(shared)
    b0 = c * NBL
    m["x"] = np.ascontiguousarray(np.asarray(inputs["x"], dtype=np.float32)[b0:b0 + NBL])
    m["ctx"] = np.ascontiguousarray(np.asarray(inputs["ctx"], dtype=np.float32)[b0:b0 + NBL])
    cv = np.stack([np.asarray(inputs["c"])[b0], np.asarray(inputs["c"])[b0 + 1], np.asarray(inputs["c_ctx"])], axis=0)
    m["cT"] = np.ascontiguousarray(cv.astype(np.float32).reshape(3, NCH, 128).transpose(2, 1, 0))
    return m


def kernel(**inputs):
    kb = K()
    nc = kb.build()
    shared = host_prep(inputs)
    in_maps = [core_inputs(inputs, shared, c) for c in range(8)]
    in_maps = [{k: v for k, v in m.items() if k in kb.din} for m in in_maps]
    res = run_bass_kernel_spmd(nc, in_maps, core_ids=list(range(8)))
    return np.concatenate([np.asarray(r["out"]) for r in res.results], axis=0).astype(np.float32)
```
